# Optimizing a Trainium2 kernel written in Bass

```python
import math, functools
import jax, jax.numpy as jnp
from jax import lax
import numpy as np

D_MODEL = 1024
BATCH = 16
SEQ = 2048
DEPTH = 1

PLE_DIM = 256
M_HEADS = 4
M_WIDTH = D_MODEL
M_HEAD_DIM = M_WIDTH // M_HEADS
QKV_BLOCK = 4
CONV_WIDTH = 4
M_CHUNK = 64
G_GROUPS = 8
G_WIDTH = D_MODEL
G_GROUP_DIM = G_WIDTH // G_GROUPS
G_CHUNK = 128
D_FF = 4 * D_MODEL
IN_COLS = 2 * M_WIDTH + 2 * G_WIDTH + 2 * D_MODEL
EPS = 1e-6

kernel_name = "hybrid_mlstm_gmlp_gated_block"


def rms_norm(x, g):
    x32 = x.astype(jnp.float32)
    y = x32 * lax.rsqrt(jnp.mean(x32 * x32, axis=-1, keepdims=True) + EPS)
    return (y * g.astype(jnp.float32)).astype(x.dtype)


def layer_norm(x, w, b=None):
    x32 = x.astype(jnp.float32)
    mu = jnp.mean(x32, axis=-1, keepdims=True)
    var = jnp.mean(jnp.square(x32 - mu), axis=-1, keepdims=True)
    y = (x32 - mu) * lax.rsqrt(var + EPS) * w.astype(jnp.float32)
    if b is not None:
        y = y + b.astype(jnp.float32)
    return y.astype(x.dtype)


def causal_conv(x, w, b):
    K = w.shape[0]
    S = x.shape[1]
    xp = jnp.pad(x, ((0, 0), (K - 1, 0), (0, 0)))
    out = xp[:, 0:S] * w[0]
    for j in range(1, K):
        out = out + xp[:, j:j + S] * w[j]
    return out + b


def block_diag_proj(x, w):
    B, S, C = x.shape
    nb, bs, _ = w.shape
    return jnp.einsum('bsnd,nde->bsne', x.reshape(B, S, nb, bs), w).reshape(B, S, C)


def mlstm_chunkwise(q, k, v, ig, lf):
    out_dtype = q.dtype
    B, S, H, dk = q.shape
    dv = v.shape[-1]
    L = M_CHUNK
    NC = S // L

    def chunks(t):
        return t.astype(jnp.float32).reshape(B, NC, L, H, -1).transpose(1, 0, 3, 2, 4)

    def gchunks(t):
        return t.astype(jnp.float32).reshape(B, NC, L, H).transpose(1, 0, 3, 2)

    qc, kc, vc = chunks(q), chunks(k) * (dk ** -0.5), chunks(v)
    igc, lfc = gchunks(ig), gchunks(lf)
    mask = jnp.tril(jnp.ones((L, L), dtype=bool))

    def step(carry, inp):
        C, n, m = carry
        qt, kt, vt, it, ft = inp
        b = jnp.cumsum(ft, axis=-1)
        D = b[..., :, None] - b[..., None, :] + it[..., None, :]
        D = jnp.where(mask, D, -jnp.inf)
        a = b + m[..., None]
        m_t = jnp.maximum(a, jnp.max(D, axis=-1))
        Dw = jnp.exp(D - m_t[..., None])
        inter_w = jnp.exp(a - m_t)
        Sqk = jnp.einsum('bhtd,bhsd->bhts', qt, kt) * Dw
        num = inter_w[..., None] * jnp.einsum('bhtd,bhde->bhte', qt, C) \
            + jnp.einsum('bhts,bhse->bhte', Sqk, vt)
        den = inter_w * jnp.einsum('bhtd,bhd->bht', qt, n) + jnp.sum(Sqk, axis=-1)
        h = num / jnp.maximum(jnp.abs(den), jnp.exp(-m_t))[..., None]
        bL = b[..., -1]
        w_s = bL[..., None] - b + it
        m_new = jnp.maximum(bL + m, jnp.max(w_s, axis=-1))
        decay = jnp.exp(bL + m - m_new)
        ws = jnp.exp(w_s - m_new[..., None])
        C_new = decay[..., None, None] * C + jnp.einsum('bhsd,bhse->bhde', kt * ws[..., None], vt)
        n_new = decay[..., None] * n + jnp.einsum('bhs,bhsd->bhd', ws, kt)
        return (C_new, n_new, m_new), h

    init = (jnp.zeros((B, H, dk, dv), jnp.float32),
            jnp.zeros((B, H, dk), jnp.float32),
            jnp.zeros((B, H), jnp.float32))
    _, hc = lax.scan(step, init, (qc, kc, vc, igc, lfc))
    return hc.transpose(1, 0, 3, 2, 4).reshape(B, S, H, dv).astype(out_dtype)


def token_mixer(h, w_in, conv_w, conv_b, wq_bd, wk_bd, wv_bd, w_if, b_if, m_norm_g, m_skip,
                g_ln_w, g_ln_b, w_spatial, b_spatial, w_br_m, w_br_g, w_out):
    B, S, _ = h.shape
    proj = h @ w_in
    m_x, m_z, g_uv, gate_pre = jnp.split(
        proj, [M_WIDTH, 2 * M_WIDTH, 2 * M_WIDTH + 2 * G_WIDTH], axis=-1)

    m_c = jax.nn.silu(causal_conv(m_x, conv_w, conv_b))
    q = block_diag_proj(m_c, wq_bd)
    k = block_diag_proj(m_c, wk_bd)
    v = block_diag_proj(m_x, wv_bd)
    gates = jnp.concatenate([q, k, v], axis=-1) @ w_if + b_if
    ig, f_pre = jnp.split(gates.astype(jnp.float32), 2, axis=-1)
    lf = jax.nn.log_sigmoid(f_pre)
    heads = lambda t: t.reshape(B, S, M_HEADS, M_HEAD_DIM)
    hm = mlstm_chunkwise(heads(q), heads(k), heads(v), ig, lf)
    hm = layer_norm(hm, jnp.ones((M_HEAD_DIM,), h.dtype)).reshape(B, S, M_WIDTH) * m_norm_g
    y_m = (hm + m_skip * m_c) * jax.nn.silu(m_z)

    g_u, g_v = jnp.split(jax.nn.gelu(g_uv), 2, axis=-1)
    g_v = layer_norm(g_v, g_ln_w, g_ln_b)
    vv = g_v.reshape(B, S // G_CHUNK, G_CHUNK, G_GROUPS, G_GROUP_DIM)
    w_causal = jnp.where(jnp.tril(jnp.ones((G_CHUNK, G_CHUNK), dtype=bool)), w_spatial, 0)
    mixed = jnp.einsum('gts,bcsgd->bctgd', w_causal, vv) + b_spatial.T[None, None, :, :, None]
    y_g = g_u * mixed.reshape(B, S, G_WIDTH)

    gate_m, gate_g = jnp.split(jax.nn.sigmoid(gate_pre), 2, axis=-1)
    merged = gate_m * (y_m @ w_br_m) + gate_g * (y_g @ w_br_g)
    return merged @ w_out


def setup_inputs(seed: int = 0) -> dict:
    key = jax.random.key(seed)
    ks = jax.random.split(key, 32)
    f32 = jnp.float32
    nrm = lambda k, shape, scale: jax.random.normal(k, shape, f32) * scale
    gain = lambda k, n: 1.0 + 0.01 * jax.random.normal(k, (DEPTH, n), f32)
    forget_bias = jnp.linspace(3.0, 6.0, M_HEADS, dtype=f32)
    b_if = jnp.concatenate([
        nrm(ks[10], (DEPTH, M_HEADS), 0.1),
        forget_bias[None, :] + nrm(ks[11], (DEPTH, M_HEADS), 0.1)], axis=-1)
    return {
        "x": jax.random.normal(ks[0], (BATCH, SEQ, D_MODEL), f32),
        "p": jax.random.normal(ks[1], (DEPTH, BATCH, SEQ, PLE_DIM), f32),
        "mix_pre_g": gain(ks[2], D_MODEL),
        "w_in": nrm(ks[3], (DEPTH, D_MODEL, IN_COLS), D_MODEL ** -0.5),
        "conv_w": nrm(ks[4], (DEPTH, CONV_WIDTH, M_WIDTH), CONV_WIDTH ** -0.5),
        "conv_b": nrm(ks[5], (DEPTH, M_WIDTH), 0.02),
        "wq_bd": nrm(ks[6], (DEPTH, M_WIDTH // QKV_BLOCK, QKV_BLOCK, QKV_BLOCK), QKV_BLOCK ** -0.5),
        "wk_bd": nrm(ks[7], (DEPTH, M_WIDTH // QKV_BLOCK, QKV_BLOCK, QKV_BLOCK), QKV_BLOCK ** -0.5),
        "wv_bd": nrm(ks[8], (DEPTH, M_WIDTH // QKV_BLOCK, QKV_BLOCK, QKV_BLOCK), QKV_BLOCK ** -0.5),
        "w_if": nrm(ks[9], (DEPTH, 3 * M_WIDTH, 2 * M_HEADS), 0.5 * (3 * M_WIDTH) ** -0.5),
        "b_if": b_if,
        "m_norm_g": gain(ks[12], M_WIDTH),
        "m_skip": gain(ks[13], M_WIDTH),
        "g_ln_w": gain(ks[14], G_WIDTH),
        "g_ln_b": nrm(ks[15], (DEPTH, G_WIDTH), 0.02),
        "w_spatial": nrm(ks[16], (DEPTH, G_GROUPS, G_CHUNK, G_CHUNK), G_CHUNK ** -0.5),
        "b_spatial": 1.0 + nrm(ks[17], (DEPTH, G_GROUPS, G_CHUNK), 0.01),
        "w_br_m": nrm(ks[18], (DEPTH, M_WIDTH, D_MODEL), M_WIDTH ** -0.5),
        "w_br_g": nrm(ks[19], (DEPTH, G_WIDTH, D_MODEL), G_WIDTH ** -0.5),
        "w_out": nrm(ks[20], (DEPTH, D_MODEL, D_MODEL), D_MODEL ** -0.5),
        "mix_post_g": gain(ks[21], D_MODEL),
        "mlp_pre_g": gain(ks[22], D_MODEL),
        "w_up": nrm(ks[23], (DEPTH, D_MODEL, D_FF), D_MODEL ** -0.5),
        "w_down": nrm(ks[24], (DEPTH, D_FF, D_MODEL), D_FF ** -0.5),
        "mlp_post_g": gain(ks[25], D_MODEL),
        "ple_pre_g": gain(ks[26], D_MODEL),
        "w_ple_gate": nrm(ks[27], (DEPTH, D_MODEL, D_MODEL), D_MODEL ** -0.5),
        "w_ple_proj": nrm(ks[28], (DEPTH, PLE_DIM, D_MODEL), PLE_DIM ** -0.5),
        "ple_post_g": gain(ks[29], D_MODEL),
    }


def reference(x, p, mix_pre_g, w_in, conv_w, conv_b, wq_bd, wk_bd, wv_bd, w_if, b_if,
              m_norm_g, m_skip, g_ln_w, g_ln_b, w_spatial, b_spatial, w_br_m, w_br_g, w_out,
              mix_post_g, mlp_pre_g, w_up, w_down, mlp_post_g, ple_pre_g, w_ple_gate,
              w_ple_proj, ple_post_g):
    for i in range(DEPTH):
        h = rms_norm(x, mix_pre_g[i])
        y = token_mixer(h, w_in[i], conv_w[i], conv_b[i], wq_bd[i], wk_bd[i], wv_bd[i],
                        w_if[i], b_if[i], m_norm_g[i], m_skip[i], g_ln_w[i], g_ln_b[i],
                        w_spatial[i], b_spatial[i], w_br_m[i], w_br_g[i], w_out[i])
        x = x + rms_norm(y, mix_post_g[i])
        h = rms_norm(x, mlp_pre_g[i])
        y = jnp.square(jax.nn.relu(h @ w_up[i])) @ w_down[i]
        x = x + rms_norm(y, mlp_post_g[i])
        h = rms_norm(x, ple_pre_g[i])
        y = jax.nn.sigmoid(h @ w_ple_gate[i]) * (p[i] @ w_ple_proj[i])
        x = x + rms_norm(y, ple_post_g[i])
    return x
```

```python
import math
from contextlib import ExitStack
import numpy as np
import concourse.bass as bass
import concourse.mybir as mybir
from concourse.bass_utils import run_bass_kernel_spmd

F32 = mybir.dt.float32
BF16 = mybir.dt.bfloat16
AF = mybir.ActivationFunctionType
ALU = mybir.AluOpType

D = 1024
SEQ = 2048
NCORE = 8
TOK_CORE = 4096
TS = 512
EPS = 1e-6
LN16 = math.log(16.0)
NW = 4


class Buf:
    __slots__ = ("name", "lw", "rd", "al", "excl")

    def __init__(self, name):
        self.name = name
        self.lw = None
        self.rd = {}
        self.al = []
        self.excl = False


class SemC:
    def __init__(self, h, name):
        self.h = h
        self.count = 0
        self.name = name


class Eng:
    def __init__(self, name, sem):
        self.name = name
        self.sem = sem
        self.prog = []
        self.known = {}


class Tracker:
    def __init__(self, nc, stack):
        self.nc = nc
        self.stack = stack
        self.engs = {}
        for n in ("tensor", "scalar", "vector", "gpsimd", "sync"):
            self.engs[n] = Eng(n, self.new_sem("e_" + n))
        self.same_sync = {"scalar", "vector", "gpsimd"}

    def new_sem(self, name):
        h = self.stack.enter_context(self.nc.semaphore(name))
        return SemC(h, name)

    @staticmethod
    def _expand(bufs):
        flat = []

        def fl(x):
            if isinstance(x, (list, tuple)):
                for y in x:
                    fl(y)
            else:
                flat.append(x)
        fl(bufs)
        out = []
        seen = set()
        for b in flat:
            for c in [b] + b.al:
                if id(c) not in seen:
                    seen.add(id(c))
                    out.append(c)
        return out

    def _waits(self, E, reads, writes):
        deps = {}

        def add(tk):
            if tk is None:
                return
            s, v = tk
            if deps.get(s, 0) < v:
                deps[s] = v
        for b in reads:
            add(b.lw)
            if b.excl:
                for s, v in b.rd.items():
                    if s is not E.sem:
                        add((s, v))
        for b in writes:
            add(b.lw)
            for s, v in b.rd.items():
                add((s, v))
        for s, v in deps.items():
            if E.known.get(s, 0) >= v:
                continue
            if s is E.sem and E.name not in self.same_sync:
                continue
            E.prog.append(("wait", s, v))
            E.known[s] = v

    @staticmethod
    def _commit(tk, reads, writes):
        s, v = tk
        for b in writes:
            b.lw = tk
            b.rd = {}
        for b in reads:
            if b.rd.get(s, 0) < v:
                b.rd[s] = v

    def op(self, eng, emit, reads=(), writes=()):
        self.group(eng, [emit], reads, writes)

    def group(self, eng, emits, reads=(), writes=()):
        E = self.engs[eng]
        reads = self._expand(reads)
        writes = self._expand(writes)
        self._waits(E, reads, writes)
        for e in emits[:-1]:
            E.prog.append(("op", e, None, 0))
        E.sem.count += 1
        E.prog.append(("op", emits[-1], E.sem, 1))
        self._commit((E.sem, E.sem.count), reads, writes)

    def dma(self, eng, emits, sem, reads=(), writes=()):
        if not isinstance(emits, (list, tuple)):
            emits = [emits]
        E = self.engs[eng]
        reads = self._expand(reads)
        writes = self._expand(writes)
        self._waits(E, reads, writes)
        for e in emits:
            sem.count += 16
            E.prog.append(("op", e, sem, 16))
        self._commit((sem, sem.count), reads, writes)

    def wait_all(self, eng, bufs):
        E = self.engs[eng]
        bufs = self._expand(bufs)
        self._waits(E, bufs, bufs)

    def replay(self):
        nc = self.nc
        with nc.Block() as block:
            def mk(E):
                def body(e):
                    for it in E.prog:
                        if it[0] == "wait":
                            e.wait_ge(it[1].h, it[2])
                        else:
                            ins = getattr(e, it[1][0])(**it[1][1])
                            if it[2] is not None:
                                ins.then_inc(it[2].h, it[3])
                return body
            for n, E in self.engs.items():
                if E.prog:
                    getattr(block, n)(mk(E))


CF_CW = 0
CF_CB = 32
CF_MNG = 40
CF_SKIP = 48
CF_GPRE = 56
CF_ID = 80
CF_TRIU = 208
CF_TRIL = 336
CF_ONES = 464
CF_N = 592


def build_program(n_st=8, stop=None):
    nc = bass.Bass("TRN2", target_bir_lowering=False)
    ntok = n_st * TS

    def din(name, shape, dt=F32):
        return nc.dram_tensor(name, list(shape), dt, kind="ExternalInput").ap()

    x_d = din("x", [ntok, D])
    p_d = din("p", [ntok, 256])
    y_d = nc.dram_tensor("y", [ntok, D], F32, kind="ExternalOutput").ap()
    w_in_d = din("w_in", [D, 6144])
    w_brm_d = din("w_br_m", [D, D])
    w_brg_d = din("w_br_g", [D, D])
    w_out_d = din("w_out", [D, D])
    w_up_d = din("w_up", [D, 4096])
    w_down_d = din("w_down", [4096, D])
    w_pg_d = din("w_ple_gate", [D, D])
    w_pp_d = din("w_ple_proj", [256, D])
    cf_d = din("cf", [128, CF_N])
    bd_d = din("bd", [128, 4, 1024])
    wsp_d = din("wsp", [128, 1024])
    wif_d = din("wif", [128, 192])
    rows_d = din("rows", [7, 1024])

    def dscr(name, shape):
        return nc.dram_tensor(name, list(shape), BF16).ap()
    sc_in = dscr("sc_in", [12, 128, 4096])
    sc_brm = dscr("sc_brm", [2, 128, 4096])
    sc_brg = dscr("sc_brg", [2, 128, 4096])
    sc_out = dscr("sc_out", [2, 128, 4096])
    sc_up = dscr("sc_up", [8, 128, 4096])
    sc_down = dscr("sc_down", [8, 128, 4096])
    sc_pg = dscr("sc_pg", [2, 128, 4096])
    sc_pp = dscr("sc_pp", [1, 128, 2048])

    with ExitStack() as st:
        T = Tracker(nc, st)

        def sb(name, shape, dt):
            return st.enter_context(nc.sbuf_tensor("sb_" + name, list(shape), dt))

        cf = sb("cf", [128, CF_N], F32)
        bc = sb("bc", [128, 6, 1024], F32)
        bif = sb("bif", [128, 8], F32)
        wbd = sb("wbd", [128, 3, 1024], BF16)
        wifb = sb("wifb", [128, 192], BF16)
        weffv = sb("weffv", [128, 64], BF16)
        wct = sb("wct", [128, 1024], BF16)
        diagw = sb("diagw", [128, 32, 128], BF16)
        identb = sb("identb", [128, 128], BF16)
        Cf = sb("Cf", [128, 8, 257], F32)
        Cb = sb("Cb", [128, 8, 258], BF16)
        halo = sb("halo", [128, 8, 4], BF16)
        sm = sb("sm", [128, 384], F32)
        xres = sb("xres", [128, 4, 1024], F32)
        hT = sb("hT", [128, 8, 512], BF16)
        tmb = sb("tmb", [128, 2, 1024], BF16)
        junk = sb("junk", [128, 1024], BF16)
        junk2 = sb("junk2", [128, 1024], BF16)
        R1 = sb("R1", [128, 16512], BF16)
        R2 = sb("R2", [128, 16384], BF16)
        R3 = sb("R3", [128, 8192], BF16)
        hh = sb("hh", [128, 4, 256], F32)
        sts = sb("sts", [128, 8, 128], BF16)
        wsl = [sb(f"wsl{i}", [128, 4096], BF16) for i in range(NW)]
        pbank = [st.enter_context(nc.psum_tensor(f"pb{i}", [128, 512], F32)) for i in range(8)]

        _smo = [0]

        def smalloc(n):
            o = _smo[0]
            _smo[0] += n
            assert _smo[0] <= 384
            return sm[:, o:o + n]
        ss = smalloc(4); rs = smalloc(4); mh4 = smalloc(4); ss2 = smalloc(8)
        gt4 = smalloc(32).rearrange("p (i j) -> p i j", i=4)
        e14 = smalloc(16).rearrange("p (i j) -> p i j", i=4)
        lfn4 = smalloc(16).rearrange("p (i j) -> p i j", i=4)
        args4 = smalloc(80).rearrange("p (i j) -> p i j", i=4)
        EE4 = smalloc(80).rearrange("p (i j) -> p i j", i=4)
        ad = smalloc(4); mm4 = smalloc(4); r4 = smalloc(4); nmr4 = smalloc(4)
        bst = smalloc(48); mv = smalloc(8); lnr = smalloc(4); mvg = smalloc(2); lng = smalloc(2)
        bst_v = bst.rearrange("p (a b) -> p a b", b=6)

        bufs = {}
        regions = {}

        def LB(name, region=None, lo=0, hi=0):
            b = Buf(name)
            bufs[name] = b
            if region is not None:
                for (ob, olo, ohi) in regions.setdefault(region, []):
                    if lo < ohi and olo < hi:
                        b.al.append(ob)
                        ob.al.append(b)
                regions[region].append((b, lo, hi))
            return b

        mc = R1[:, 0:4096].rearrange("p (k n) -> p k n", k=8)
        sz = R1[:, 4096:8192].rearrange("p (k n) -> p k n", k=8)
        gu = R1[:, 8192:12288].rearrange("p (k n) -> p k n", k=8)
        mx = R1[:, 12288:12288 + 4128].rearrange("p (k n) -> p k n", k=8)
        act = R1[:, 0:16384].rearrange("p (k n) -> p k n", k=32)
        Bmc = [LB(f"mc{k}", "R1", k * 512, (k + 1) * 512) for k in range(8)]
        Bsz = [LB(f"sz{k}", "R1", 4096 + k * 512, 4096 + (k + 1) * 512) for k in range(8)]
        Bgu = [LB(f"gu{k}", "R1", 8192 + k * 512, 8192 + (k + 1) * 512) for k in range(8)]
        Bmx = [LB(f"mx{k}", "R1", 12288 + k * 516, 12288 + (k + 1) * 516) for k in range(8)]
        Bact = [LB(f"act{c}", "R1", c * 512, (c + 1) * 512) for c in range(32)]
        gvf = R2[:, 0:4096].bitcast(F32).rearrange("p (i d) -> p i d", i=2)
        gv = R2[:, 4096:8192].rearrange("p (i d) -> p i d", i=4)
        tmix = R2[:, 8192:9216].bitcast(F32)
        wvt_t = R2[:, 0:1024]
        wsp_t = R2[:, 1024:2048]
        qT = R2[:, 0:4096].rearrange("p (k n) -> p k n", k=8)
        kT = R2[:, 4096:8192].rearrange("p (k n) -> p k n", k=8)
        kTM = R2[:, 8192:10240].rearrange("p (i d) -> p i d", i=2)
        vt = R2[:, 10240:12320].rearrange("p (i h d) -> p i h d", i=2, h=4)
        vh = R2[:, 12320:14400].rearrange("p (i h d) -> p i h d", i=2, h=4)
        gmh = R2[:, 0:2048].rearrange("p (k n) -> p k n", k=4)
        ggh = R2[:, 2048:4096].rearrange("p (k n) -> p k n", k=4)
        mrg = R2[:, 4096:8192].rearrange("p (k n) -> p k n", k=8)
        t12 = R2[:, 8192:12288].bitcast(F32).rearrange("p (i n) -> p i n", i=4)
        sqt = R2[:, 0:2048].bitcast(F32).rearrange("p (i n) -> p i n", i=2)
        pf32 = R2[:, 0:2048].bitcast(F32).rearrange("p (i n) -> p i n", i=4)
        pT = R2[:, 2048:3072].rearrange("p (k n) -> p k n", k=2)
        sg = R2[:, 4096:6144].bitcast(F32).rearrange("p (i n) -> p i n", i=2)
        xnext = R2[:, 8192:16384].bitcast(F32).rearrange("p (i d) -> p i d", i=4)
        Bxn = [LB(f"xn{i}", "R2", 8192 + i * 2048, 8192 + (i + 1) * 2048) for i in range(4)]
        Bgvf = [LB(f"gvf{i}", "R2", i * 2048, (i + 1) * 2048) for i in range(2)]
        Bgv = [LB(f"gv{i}", "R2", 4096 + i * 1024, 4096 + (i + 1) * 1024) for i in range(4)]
        Btmix = LB("tmix", "R2", 8192, 9216)
        Bwvt = LB("wvt_t", "R2", 0, 1024); Bwsp = LB("wsp_t", "R2", 1024, 2048)
        BqT = [LB(f"qT{k}", "R2", k * 512, (k + 1) * 512) for k in range(8)]
        BkT = [LB(f"kT{k}", "R2", 4096 + k * 512, 4096 + (k + 1) * 512) for k in range(8)]
        BkTM = [LB(f"kTM{i}", "R2", 8192 + i * 1024, 8192 + (i + 1) * 1024) for i in range(2)]
        Bvt = [[LB(f"vt{i}_{h}", "R2", 10240 + i * 1040 + h * 260, 10240 + i * 1040 + (h + 1) * 260) for h in range(4)]
               for i in range(2)]
        Bvh = [[LB(f"vh{i}_{h}", "R2", 12320 + i * 1040 + h * 260, 12320 + i * 1040 + (h + 1) * 260) for h in range(4)]
               for i in range(2)]
        Bgmh = LB("gmh", "R2", 0, 2048); Bggh = LB("ggh", "R2", 2048, 4096)
        Bmrg = LB("mrg", "R2", 4096, 8192)
        Bt12 = [LB(f"t12_{i}", "R2", 8192 + i * 1024, 8192 + (i + 1) * 1024) for i in range(4)]
        Bsqt = [LB(f"sqt{i}", "R2", i * 1024, (i + 1) * 1024) for i in range(2)]
        Bpf32 = LB("pf32", "R2", 0, 2048); BpT = LB("pT", "R2", 2048, 3072)
        Bsg = [LB(f"sg{i}", "R2", 4096 + i * 1024, 4096 + (i + 1) * 1024) for i in range(2)]
        szg = R3[:, 0:4096].rearrange("p (k n) -> p k n", k=8)
        skz = R3[:, 4096:8192].rearrange("p (k n) -> p k n", k=8)
        ybuf = R3[:, 0:8192].bitcast(F32).rearrange("p (i d) -> p i d", i=4)
        Bszg = [LB(f"szg{k}", "R3", k * 512, (k + 1) * 512) for k in range(8)]
        Bskz = [LB(f"skz{k}", "R3", 4096 + k * 512, 4096 + (k + 1) * 512) for k in range(8)]
        Bybuf = [LB(f"ybuf{i}", "R3", i * 2048, (i + 1) * 2048) for i in range(4)]
        Bcf = LB("cf"); Bbc = LB("bc"); Bbif = LB("bif"); Bwbd = LB("wbd"); Bwifb = LB("wifb")
        Bweffv = LB("weffv"); Bwct = LB("wct"); Bdiagw = LB("diagw"); Bidentb = LB("identb")
        BCf = [LB(f"Cf{i}") for i in range(8)]; BCb = [LB(f"Cb{i}") for i in range(8)]
        Bhalo = LB("halo")
        Bx = [LB(f"x{i}") for i in range(4)]
        BhT = [LB(f"hT{i}") for i in range(4)]; Btmb = [LB("tmb0"), LB("tmb1")]; Bjh = [LB("junk_a"), LB("junk_b")]; Bjunk = Bjh; Bjunk2 = LB("junk2")
        Bhh = [LB(f"hh{i}") for i in range(4)]; Bsts = [LB(f"sts{i}") for i in range(8)]
        Bw = [LB(f"w{i}") for i in range(NW)]
        Bp = [LB(f"ps{i}") for i in range(8)]
        for b_ in Bp:
            b_.excl = True
        Bout = LB("out")
        Bsm = {n: LB("sm_" + n) for n in ["ss", "rs", "mh4", "ss2", "gt", "e1", "lfn", "args", "EE", "EE0", "EE1", "EE2", "EE3", "ad", "nmr",
                                            "mm4", "r4", "bst", "mv", "lnr", "mvg", "lng"]}
        NSC = {"in": 12, "brm": 2, "brg": 2, "out": 2, "up": 8, "down": 8, "pg": 2, "pp": 1}
        Bsm["ss"] = [LB(f"sm_ss{i}") for i in range(4)]
        Bsm["ss2"] = [LB(f"sm_ss2{i}") for i in range(8)]
        Bsm["ad"] = [LB(f"sm_ad{i}") for i in range(4)]
        Bsm["bst"] = [LB(f"sm_bst{i}") for i in range(8)]
        Bsc = {n: [LB(f"sc_{n}{u}") for u in range(k)] for n, k in NSC.items()}
        NCS = 6
        Bthr = [LB(f"thr{i}") for i in range(NCS)]

        s_const = T.new_sem("s_const")
        s_cast = [T.new_sem(f"s_cast{i}") for i in range(NCS)]
        s_w = [T.new_sem(f"s_w{i}") for i in range(NW)]
        s_x = T.new_sem("s_x"); s_p = T.new_sem("s_p"); s_st = T.new_sem("s_st")
        s_cc = T.new_sem("s_cc")
        s_xc = T.new_sem("s_xc")

        _pi = [0]

        pinned = set()

        def pnext():
            while True:
                i = _pi[0] % 8
                _pi[0] += 1
                if i not in pinned:
                    return pbank[i], Bp[i]

        def pin(pbuf):
            pinned.add(Bp.index(pbuf))

        def unpin(pbuf):
            pinned.discard(Bp.index(pbuf))

        def mm(out_ap, pairs, reads, pbuf, first=True, last=True):
            n = len(pairs)
            emits = []
            for i, (l, r) in enumerate(pairs):
                emits.append(("matmul", dict(out=out_ap, lhsT=l, rhs=r, start=(first and i == 0),
                                             stop=(last and i == n - 1))))
            T.group("tensor", emits, reads=reads, writes=[pbuf])

        def MM1(out, lhsT, rhs):
            return ("matmul", dict(out=out, lhsT=lhsT, rhs=rhs, start=True, stop=True))

        def TR(out, in_):
            return ("transpose", dict(out=out, in_=in_, identity=identb[:]))

        def DMA(out, in_):
            return ("dma_start", dict(out=out, in_=in_))

        def V(name, reads, writes, **kw):
            T.op("vector", (name, kw), reads, writes)

        def A(name, reads, writes, **kw):
            T.op("scalar", (name, kw), reads, writes)

        def G(name, reads, writes, **kw):
            T.op("gpsimd", (name, kw), reads, writes)

        per_st = []
        for u in (6, 7, 4, 5, 0, 1, 2, 3):
            per_st.append((sc_in[u], 4096, Bsc["in"][u]))
        for half in range(2):
            per_st.append((sc_in[8 + half], 4096, Bsc["in"][8 + half]))
            per_st.append((sc_in[10 + half], 4096, Bsc["in"][10 + half]))
            per_st.append((sc_brm[half], 4096, Bsc["brm"][half]))
            per_st.append((sc_brg[half], 4096, Bsc["brg"][half]))
        for u in range(2):
            per_st.append((sc_out[u], 4096, Bsc["out"][u]))
        for u in range(8):
            per_st.append((sc_up[u], 4096, Bsc["up"][u]))
        for u in range(8):
            per_st.append((sc_down[u], 4096, Bsc["down"][u]))
        for u in range(2):
            per_st.append((sc_pg[u], 4096, Bsc["pg"][u]))
        per_st.append((sc_pp[0], 2048, Bsc["pp"][0]))
        units = []
        for _ in range(n_st):
            units.extend(per_st)
        w_issued = [0]
        w_taken = [0]

        def w_issue_upto(n):
            n = min(n, len(units))
            while w_issued[0] < n:
                j = w_issued[0]
                ap, ncol, sbuf_ = units[j]
                slot = j % NW
                T.dma("sync", [DMA(wsl[slot][:, 0:ncol], ap)], s_w[slot], reads=[sbuf_], writes=[Bw[slot]])
                w_issued[0] += 1

        def next_w():
            j = w_taken[0]
            w_taken[0] += 1
            w_issue_upto(j + 1)
            slot = j % NW
            return wsl[slot][:, 0:4096].rearrange("p (k n) -> p k n", k=8), Bw[slot], wsl[slot]

        T.dma("sync", [DMA(cf[:], cf_d[:, :])] +
              [DMA(bc[:, r, :], rows_d[r:r + 1, :].partition_broadcast(128)) for r in range(6)] +
              [DMA(bif[:], rows_d[6:7, 0:8].partition_broadcast(128))], s_const, writes=[Bcf, Bbc, Bbif])
        T.dma("gpsimd", [DMA(wbd[:, w, :], bd_d[:, w, :]) for w in range(3)] +
              [DMA(wvt_t, bd_d[:, 3, :]), DMA(wsp_t, wsp_d[:, :]), DMA(wifb[:], wif_d[:, :])],
              s_cc, writes=[Bwbd, Bwvt, Bwsp, Bwifb])

        cast_list = []

        def cast_w(name, w_ap, scr, nunits, kind, order=None):
            for u in (order or range(nunits)):
                if kind == "cols":
                    src = w_ap.rearrange("(k p) n -> p k n", p=128)[:, :, u * 512:(u + 1) * 512]
                    dst = scr[u].rearrange("p (k n) -> p k n", k=8)
                elif kind == "down":
                    c, uu = u // 4, u % 4
                    src = w_ap.rearrange("(u k p) n -> u p k n", k=8, p=128)[uu][:, :, c * 512:(c + 1) * 512]
                    dst = scr[u].rearrange("p (k n) -> p k n", k=8)
                else:
                    src = w_ap.rearrange("(k p) n -> p k n", p=128)
                    dst = scr[0].rearrange("p (k n) -> p k n", k=2)
                cast_list.append((DMA(dst, src), Bsc[name][u]))

        cast_w("in", w_in_d, sc_in, 12, "cols", order=(6, 7, 4, 5, 0, 1, 2, 3, 8, 10, 9, 11))
        cast_w("brm", w_brm_d, sc_brm, 2, "cols")
        cast_w("brg", w_brg_d, sc_brg, 2, "cols")
        cast_w("out", w_out_d, sc_out, 2, "cols")
        cast_w("up", w_up_d, sc_up, 8, "cols")
        cast_w("down", w_down_d, sc_down, 8, "down")
        cast_w("pg", w_pg_d, sc_pg, 2, "cols")
        cast_w("pp", w_pp_d, sc_pp, 1, "pp")
        cast_done = [0]

        def cast_some(k):
            for _ in range(k):
                n = cast_done[0]
                if n >= len(cast_list):
                    return
                emit, ub = cast_list[n]
                T.dma("gpsimd", [emit], s_cast[n % NCS], writes=[ub, Bthr[n % NCS]])
                cast_done[0] += 1

        cast_some(12)

        ident_f = cf[:, CF_ID:CF_ID + 128]
        triu_f = cf[:, CF_TRIU:CF_TRIU + 128]
        tril_f = cf[:, CF_TRIL:CF_TRIL + 128]
        ones_f = cf[:, CF_ONES:CF_ONES + 128]

        def col(base, k):
            return cf[:, base + k:base + k + 1]

        def k3(ap, k):
            return ap.rearrange("p (k t) -> p k t", k=k)

        V("tensor_copy", [Bcf], [Bidentb], out=identb[:], in_=ident_f)
        G("memset", [], [Bsm["mh4"]], ap=mh4, constant=-0.5)
        for j in range(4):
            for k in range(8):
                V("tensor_scalar", [Bcf], [Bdiagw], out=diagw[:, j * 8 + k, :], in0=ident_f,
                  scalar1=col(CF_CW, k * 4 + j), scalar2=None, op0=ALU.mult)
        pt, pb_ = pnext()
        ptb = pt[:].bitcast(BF16)
        for g in range(8):
            gs = slice(g * 128, (g + 1) * 128)
            V("tensor_tensor", [Bwsp, Bcf], [Bwsp], out=wsp_t[:, gs], in0=wsp_t[:, gs], in1=tril_f, op=ALU.mult)
        T.group("tensor", [TR(ptb[:, g * 128:(g + 1) * 128], wsp_t[:, g * 128:(g + 1) * 128]) for g in range(8)],
                reads=[Bwsp, Bidentb], writes=[pb_])
        V("tensor_copy", [pb_], [Bwct], out=wct[:], in_=ptb)
        pt, pb_ = pnext()
        T.group("tensor", [MM1(pt[:, kc * 8:(kc + 1) * 8], wvt_t[:, kc * 128:(kc + 1) * 128],
                               wifb[:, (16 + kc) * 8:(17 + kc) * 8]) for kc in range(8)],
                reads=[Bwvt, Bwifb], writes=[pb_])
        V("tensor_copy", [pb_], [Bweffv], out=weffv[:], in_=pt[:, 0:64])

        Wq = k3(wbd[:, 0, :], 8)
        Wk = k3(wbd[:, 1, :], 8)
        Wv = k3(wbd[:, 2, :], 8)
        wif3 = wifb[:].rearrange("p (c j) -> p c j", j=8)
        weff3 = weffv[:].rearrange("p (c j) -> p c j", j=8)
        wct3 = k3(wct[:], 8)
        bsp3 = k3(bc[:, 5, :], 8)
        s3 = ss2.rearrange("p (i c) -> p i c", c=2)
        mv3 = mv.rearrange("p (h c) -> p h c", c=2)
        hh3 = k3(hh[:].rearrange("p h d -> p (h d)"), 8)
        tmix3 = k3(tmix, 4)

        def rms_to_hT(gidx, xsrc=None, Bsrc=None):
            if xsrc is None:
                xsrc, Bsrc = xres, Bx
            for i in (0, 2, 1, 3):
                if i < 2:
                    A("activation", [Bsrc[i]], [Bjunk, Bsm["ss"][i]], out=junk[:], in_=xsrc[:, i, :], func=AF.Square,
                      accum_out=ss[:, i:i + 1])
                else:
                    V("scalar_tensor_tensor", [Bsrc[i]], [Bjunk2, Bsm["ss"][i]], out=junk2[:], in0=xsrc[:, i, :], scalar=1.0,
                      in1=xsrc[:, i, :], op0=ALU.mult, op1=ALU.mult, accum_out=ss[:, i:i + 1])
            V("tensor_scalar", [Bsm["ss"]], [Bsm["rs"]], out=rs, in0=ss, scalar1=1.0 / D, scalar2=EPS,
              op0=ALU.mult, op1=ALU.add)
            G("tensor_tensor", [Bsm["rs"], Bsm["mh4"]], [Bsm["rs"]], out=rs, in0=rs, in1=mh4, op=ALU.pow)
            gcol = CF_GPRE + gidx * 8
            gb = cf[:, gcol:gcol + 8].unsqueeze(2).to_broadcast([128, 8, 128])
            for i in range(4):
                sl = i % 2
                A("activation", [Bsrc[i], Bsm["rs"]], [Btmb[sl]], out=tmb[:, sl, :], in_=xsrc[:, i, :], func=AF.Copy,
                  scale=rs[:, i:i + 1])
                pt, pb_ = pnext()
                ptb = pt[:].bitcast(BF16)
                T.group("tensor", [TR(ptb[:, k * 128:(k + 1) * 128], tmb[:, sl, k * 128:(k + 1) * 128])
                                   for k in range(8)], reads=[Btmb[sl], Bidentb], writes=[pb_])
                V("tensor_tensor", [pb_, Bcf], [BhT[i]], out=hT[:, :, i * 128:(i + 1) * 128], in0=k3(ptb, 8), in1=gb,
                  op=ALU.mult)

        def post_norm_residual(pidx, halves, to_ybuf=False):
            if halves:
                V("tensor_tensor", [Bsm["ss2"]], [Bsm["ss"]], out=ss, in0=s3[:, :, 0], in1=s3[:, :, 1], op=ALU.add)
                src, bsrc = ss, Bsm["ss"]
            else:
                src, bsrc = ss2[:, 0:4], Bsm["ss2"]
            V("tensor_scalar", [bsrc], [Bsm["rs"]], out=rs, in0=src, scalar1=1.0 / D, scalar2=EPS,
              op0=ALU.mult, op1=ALU.add)
            G("tensor_tensor", [Bsm["rs"], Bsm["mh4"]], [Bsm["rs"]], out=rs, in0=rs, in1=mh4, op=ALU.pow)
            for i in range(4):
                V("scalar_tensor_tensor", [Bybuf[i], Bsm["rs"], Bbc], [Bybuf[i]], out=ybuf[:, i, :],
                  in0=ybuf[:, i, :], scalar=rs[:, i:i + 1], in1=bc[:, pidx, :], op0=ALU.mult, op1=ALU.mult)
                dst, dbuf = (ybuf[:, i, :], Bybuf[i]) if to_ybuf else (xres[:, i, :], Bx[i])
                (V if i % 2 == 0 else G)("tensor_tensor", [Bx[i], Bybuf[i]], [dbuf], out=dst, in0=xres[:, i, :],
                                          in1=ybuf[:, i, :], op=ALU.add)

        def tm_out_stage(lhs_tile, lhs_buf, nk_units, get_rhs, per_chunk=False):
            for c in range(2):
                banks = [pnext() for _ in range(4)]
                for u in range(nk_units):
                    w3, wb, _ = get_rhs(c, u)
                    for i in range(4):
                        pt, pb_ = banks[i]
                        mm(pt[:, :], [(lhs_tile[:, u * 8 + k, i * 128:(i + 1) * 128], w3[:, k, :]) for k in range(8)],
                           ([lhs_buf[u * 8 + k] for k in range(8)] if per_chunk else [lhs_buf]) + [wb], pb_,
                           first=(u == 0), last=(u == nk_units - 1))
                for i in range(4):
                    pt, pb_ = banks[i]
                    V("tensor_copy", [pb_], [Bybuf[i]], out=ybuf[:, i, c * 512:(c + 1) * 512], in_=pt[:, :])
                    A("activation", [Bybuf[i]], [Bjh[i % 2], Bsm["ss2"][i * 2 + c]], out=junk[:, (i % 2) * 512:(i % 2 + 1) * 512],
                      in_=ybuf[:, i, c * 512:(c + 1) * 512], func=AF.Square,
                      accum_out=ss2[:, i * 2 + c:i * 2 + c + 1])

        def load_x(sti_):
            rr = sti_ * TS
            T.dma("sync", [DMA(xnext[:, i, :], x_d[rr + i * 128:rr + (i + 1) * 128, :]) for i in range(4)],
                  s_x, writes=Bxn)

        if stop != "P":
            w_issue_upto(NW - 1)
            load_x(0)
            rms_to_hT(0, xnext, Bxn)
        for sti in range(0 if stop == "P" else n_st):
            first_of_seq = (sti % 4 == 0)
            r0 = sti * TS
            T.dma("gpsimd", [DMA(xres[:, i, :], xnext[:, i, :]) for i in range(4)], s_xc, reads=Bxn, writes=Bx)
            if first_of_seq:
                G("memset", [], BCf, ap=Cf[:], constant=0.0)
                G("memset", [], BCb, ap=Cb[:], constant=0.0)
                G("memset", [], [Bhalo], ap=halo[:], constant=0.0)
            cast_some(6)
            wvs = [next_w(), next_w()]

            def gv_mm(i):
                sl = i % 2
                for c in range(2):
                    pt, pb_ = pnext()
                    mm(pt[:, :], [(hT[:, k, i * 128:(i + 1) * 128], wvs[c][0][:, k, :]) for k in range(8)],
                       [BhT, wvs[c][1]], pb_)
                    A("activation", [pb_], [Bgvf[sl]], out=gvf[:, sl, c * 512:(c + 1) * 512], in_=pt[:, :],
                      func=AF.Gelu_apprx_tanh)

            def gv_ln(i):
                sl = i % 2
                for c in range(2):
                    V("bn_stats", [Bgvf[sl]], [Bsm["bst"][c]], out=bst_v[:, c, :], in_=gvf[:, sl, c * 512:(c + 1) * 512])
                V("bn_aggr", Bsm["bst"][0:2], [Bsm["mvg"]], out=mvg, in_=bst[:, 0:12])
                V("tensor_scalar", [Bsm["mvg"]], [Bsm["lng"]], out=lng[:, 0:1], in0=mvg[:, 1:2], scalar1=EPS,
                  scalar2=None, op0=ALU.add)
                G("tensor_tensor", [Bsm["lng"], Bsm["mh4"]], [Bsm["lng"]], out=lng[:, 0:1], in0=lng[:, 0:1],
                  in1=mh4[:, 0:1], op=ALU.pow)
                V("tensor_scalar", [Bgvf[sl], Bsm["mvg"], Bsm["lng"]], [Bgvf[sl]], out=gvf[:, sl, :], in0=gvf[:, sl, :],
                  scalar1=mvg[:, 0:1], scalar2=lng[:, 0:1], op0=ALU.subtract, op1=ALU.mult)
                V("tensor_tensor", [Bgvf[sl], Bbc], [Bgvf[sl]], out=gvf[:, sl, :], in0=gvf[:, sl, :], in1=bc[:, 3, :],
                  op=ALU.mult)
                V("tensor_tensor", [Bgvf[sl], Bbc], [Bgv[i]], out=gv[:, i, :], in0=gvf[:, sl, :], in1=bc[:, 4, :],
                  op=ALU.add)

            def gv_mix(i):
                for hf in range(2):
                    pt, pb_ = pnext()
                    T.group("tensor", [MM1(pt[:, j * 128:(j + 1) * 128],
                                           gv[:, i, (hf * 4 + j) * 128:(hf * 4 + j + 1) * 128],
                                           wct3[:, hf * 4 + j, :]) for j in range(4)],
                            reads=[Bgv[i], Bwct], writes=[pb_])
                    V("tensor_tensor", [pb_, Bbc], [Btmix], out=tmix3, in0=k3(pt[:, :], 4),
                      in1=bsp3[:, hf * 4:(hf + 1) * 4, :], op=ALU.add)
                    gus = gu[:, hf * 4:(hf + 1) * 4, i * 128:(i + 1) * 128]
                    G("tensor_tensor", [Btmix, Bgu[hf * 4:(hf + 1) * 4]], [Bgu[hf * 4:(hf + 1) * 4]], out=gus, in0=tmix3, in1=gus, op=ALU.mult)

            def fm_unit(u):
                w3, wb, _ = next_w()
                grp = u // 2
                for m in range(4):
                    ch = (u % 2) * 4 + m
                    pt, pb_ = pnext()
                    mm(pt[:, :], [(w3[:, k, m * 128:(m + 1) * 128], hT[:, k, :]) for k in range(8)], [wb, BhT], pb_)
                    if grp == 0:
                        A("activation", [pb_], [Bmx[ch]], out=mx[:, ch, 4:516], in_=pt[:, :], func=AF.Identity)
                    elif grp == 1:
                        A("activation", [pb_], [Bsz[ch]], out=sz[:, ch, :], in_=pt[:, :], func=AF.Silu)
                    else:
                        A("activation", [pb_], [Bgu[ch]], out=gu[:, ch, :], in_=pt[:, :], func=AF.Gelu_apprx_tanh)

            gv_mm(0); gv_mm(1)
            fm_unit(4)
            gv_ln(0); gv_ln(1)
            gv_mm(2); gv_mm(3)
            fm_unit(5)
            gv_ln(2); gv_ln(3)
            fm_unit(0)
            gv_mix(0); gv_mix(1)
            fm_unit(1)
            gv_mix(2); gv_mix(3)
            fm_unit(2)
            fm_unit(3)
            G("tensor_copy", [Bhalo], [Bmx], out=mx[:, :, 1:4], in_=halo[:, :, 0:3])
            for k in range(8):
                pt, pb_ = pnext()
                mm(pt[:, :], [(diagw[:, j * 8 + k, :], mx[:, k, 1 + j:513 + j]) for j in range(4)], [Bdiagw, Bmx[k]], pb_)
                A("activation", [pb_, Bcf], [Bmc[k]], out=mc[:, k, :], in_=pt[:, :], func=AF.Silu, bias=col(CF_CB, k))
            if stop == "S4":
                break
            cast_some(8)
            for k in range(8):
                pt, pb_ = pnext()
                mm(pt[:, :], [(Wq[:, k, :], mc[:, k, :])], [Bwbd, Bmc[k]], pb_)
                V("tensor_copy", [pb_], [BqT[k]], out=qT[:, k, :], in_=pt[:, :])
                pt, pb_ = pnext()
                mm(pt[:, :], [(Wk[:, k, :], mc[:, k, :])], [Bwbd, Bmc[k]], pb_)
                A("activation", [pb_], [BkT[k]], out=kT[:, k, :], in_=pt[:, :], func=AF.Identity)
            for k in range(8):
                V("tensor_scalar", [Bsz[k], Bcf], [Bszg[k]], out=szg[:, k, :], in0=sz[:, k, :], scalar1=col(CF_MNG, k),
                  scalar2=None, op0=ALU.mult)
                V("scalar_tensor_tensor", [Bmc[k], Bsz[k], Bcf], [Bskz[k]], out=skz[:, k, :], in0=mc[:, k, :],
                  scalar=col(CF_SKIP, k), in1=sz[:, k, :], op0=ALU.mult, op1=ALU.mult)
            pt, pb_ = pnext()
            for i in range(4):
                tsl = slice(i * 128, (i + 1) * 128)
                xsl = slice(4 + i * 128, 4 + (i + 1) * 128)
                pairs = [(qT[:, kc, tsl], wif3[:, kc, :]) for kc in range(8)]
                pairs += [(kT[:, kc, tsl], wif3[:, 8 + kc, :]) for kc in range(8)]
                pairs += [(mx[:, kc, xsl], weff3[:, kc, :]) for kc in range(8)]
                mm(pt[:, i * 8:(i + 1) * 8], pairs, [BqT, BkT, Bmx, Bwifb, Bweffv], pb_)
            V("tensor_tensor", [pb_, Bbif], [Bsm["gt"]], out=gt4, in0=pt[:, 0:32].rearrange("p (i j) -> p i j", i=4),
              in1=bif[:].unsqueeze(1).to_broadcast([128, 4, 8]), op=ALU.add)
            A("activation", [Bsm["gt"]], [Bsm["e1"]], out=e14, in_=gt4[:, :, 4:8], func=AF.Exp, scale=-1.0)
            A("activation", [Bsm["e1"]], [Bsm["lfn"]], out=lfn4, in_=e14, func=AF.Ln, bias=1.0)
            pt2, pb2 = pnext()
            T.group("tensor", [MM1(pt2[:, i * 8:i * 8 + 4], triu_f, lfn4[:, i, :]) for i in range(4)] +
                    [MM1(pt2[:, i * 8 + 4:i * 8 + 8], ones_f, lfn4[:, i, :]) for i in range(4)],
                    reads=[Bcf, Bsm["lfn"]], writes=[pb2])
            p2v = pt2[:, 0:32].rearrange("p (i j) -> p i j", i=4)
            Ba = Bsm["args"]
            V("tensor_scalar", [pb2], [Ba], out=args4[:, :, 0:4], in0=p2v[:, :, 0:4], scalar1=-1.0, scalar2=None,
              op0=ALU.mult)
            V("scalar_tensor_tensor", [pb2, Bsm["gt"]], [Ba], out=args4[:, :, 4:8], in0=gt4[:, :, 0:4], scalar=-LN16,
              in1=p2v[:, :, 0:4], op0=ALU.add, op1=ALU.add)
            V("tensor_tensor", [pb2, Ba], [Ba], out=args4[:, :, 8:12], in0=args4[:, :, 4:8], in1=p2v[:, :, 4:8],
              op=ALU.subtract)
            V("tensor_scalar", [pb2], [Ba], out=args4[:, :, 12:16], in0=p2v[:, :, 4:8], scalar1=-1.0, scalar2=None,
              op0=ALU.mult)
            V("tensor_copy", [pb2], [Ba], out=args4[:, :, 16:20], in_=p2v[:, :, 0:4])
            A("activation", [Ba], [Bsm["EE"]], out=EE4, in_=args4, func=AF.Exp)
            BE = Bsm["EE"]
            if stop == "S5a":
                break

            def A2(i):
                sl = i % 2
                EE = EE4[:, i, :]
                tsl = slice(i * 128, (i + 1) * 128)
                xsl = slice(4 + i * 128, 4 + (i + 1) * 128)
                for hf in range(2):
                    pt, pb_ = pnext()
                    T.group("tensor", [MM1(pt[:, j * 128:(j + 1) * 128], mc[:, hf * 4 + j, tsl], Wk[:, hf * 4 + j, :])
                                       for j in range(4)], reads=[Bmc, Bwbd], writes=[pb_])
                    A("activation", [pb_], [BkTM[sl]], out=kTM[:, sl, hf * 512:(hf + 1) * 512], in_=pt[:, :],
                      func=AF.Identity)
                    pt, pb_ = pnext()
                    T.group("tensor", [MM1(pt[:, j * 128:(j + 1) * 128], mx[:, hf * 4 + j, xsl], Wv[:, hf * 4 + j, :])
                                       for j in range(4)], reads=[Bmx, Bwbd], writes=[pb_])
                    for hl in range(2):
                        h = hf * 2 + hl
                        A("activation", [pb_, BE], [Bvt[sl][h]], out=vt[:, sl, h, 0:256],
                          in_=pt[:, hl * 256:(hl + 1) * 256], func=AF.Copy, scale=EE[:, 4 + h:5 + h])
                    for hl in range(2):
                        h = hf * 2 + hl
                        A("activation", [pb_, BE], [Bvh[sl][h]], out=vh[:, sl, h, 0:256],
                          in_=pt[:, hl * 256:(hl + 1) * 256], func=AF.Copy, scale=EE[:, 8 + h:9 + h])
                V("tensor_copy", [BE], Bvt[sl], out=vt[:, sl, :, 256:257], in_=EE[:, 4:8].unsqueeze(2))
                V("tensor_copy", [BE], Bvh[sl], out=vh[:, sl, :, 256:257], in_=EE[:, 8:12].unsqueeze(2))

            pN_of = {}

            def B1s(i):
                tsl = slice(i * 128, (i + 1) * 128)
                pS = []
                for h in range(4):
                    p_, pb_ = pnext()
                    mm(p_[:, 0:128], [(kT[:, 2 * h + c, tsl], qT[:, 2 * h + c, tsl]) for c in range(2)],
                       [BkT[2 * h:2 * h + 2], BqT[2 * h:2 * h + 2]], pb_)
                    pS.append((p_, pb_))
                for h in range(4):
                    sh = (i % 2) * 4 + h
                    V("tensor_tensor", [pS[h][1], Bcf], [Bsts[sh]], out=sts[:, sh, :], in0=pS[h][0][:, 0:128], in1=triu_f,
                      op=ALU.mult)

            def B1n(i):
                sl = i % 2
                tsl = slice(i * 128, (i + 1) * 128)
                pN = []
                for h in range(4):
                    sh = (i % 2) * 4 + h
                    pn, pnb = pnext()
                    mm(pn[:, 0:257], [(qT[:, 2 * h, tsl], Cb[:, 2 * h, 0:257]),
                                      (qT[:, 2 * h + 1, tsl], Cb[:, 2 * h + 1, 0:257]),
                                      (sts[:, sh, :], vt[:, sl, h, 0:257])],
                       [BqT[2 * h:2 * h + 2], BCb[2 * h], BCb[2 * h + 1], Bsts[sh], Bvt[sl][h]], pnb)
                    pN.append((pn, pnb))
                    pin(pnb)
                pN_of[i] = pN
                for h in range(4):
                    A("activation", [pN[h][1]], [Bsm["ad"][h]], out=ad[:, h:h + 1], in_=pN[h][0][:, 256:257], func=AF.Abs)

            def B1c(i):
                sl = i % 2
                EE = EE4[:, i, :]
                for h in range(4):
                    for c in range(2):
                        ci = 2 * h + c
                        pC, pCb = pnext()
                        mm(pC[:, 0:257], [(kTM[:, sl, ci * 128:(ci + 1) * 128], vh[:, sl, h, 0:257])],
                           [BkTM[sl], Bvh[sl][h]], pCb)
                        V("scalar_tensor_tensor", [pCb, BCf[ci], BE], [BCf[ci]], out=Cf[:, ci, :], in0=Cf[:, ci, :],
                          scalar=EE[:, 12 + h:13 + h], in1=pC[:, 0:257], op0=ALU.mult, op1=ALU.add)
                        if h < 2:
                            G("tensor_copy", [BCf[ci]], [BCb[ci]], out=Cb[:, ci, 0:257], in_=Cf[:, ci, :])
                        else:
                            A("activation", [BCf[ci]], [BCb[ci]], out=Cb[:, ci, 0:257], in_=Cf[:, ci, :], func=AF.Copy)

            def B2a(i):
                sl = i % 2
                EE = EE4[:, i, :]
                pN = pN_of[i]
                V("tensor_tensor", [Bsm["ad"], BE], [Bsm["mm4"]], out=mm4, in0=ad, in1=EE[:, 16:20], op=ALU.max)
                V("reciprocal", [Bsm["mm4"]], [Bsm["r4"]], out=r4, in_=mm4)
                for h in range(4):
                    pn, pnb = pN[h]
                    A("activation", [pnb, Bsm["r4"]], [Bhh[h]], out=hh[:, h, :], in_=pn[:, 0:256], func=AF.Copy,
                      scale=r4[:, h:h + 1])
                    unpin(pnb)
                for h in range(4):
                    V("bn_stats", [Bhh[h]], [Bsm["bst"][h]], out=bst_v[:, h, :], in_=hh[:, h, :])
                for h in range(4):
                    V("bn_aggr", [Bsm["bst"][h]], [Bsm["mv"]], out=mv[:, 2 * h:2 * h + 2], in_=bst[:, 6 * h:6 * h + 6])
                V("tensor_scalar", [Bsm["mv"]], [Bsm["lnr"]], out=lnr, in0=mv3[:, :, 1], scalar1=EPS, scalar2=None,
                  op0=ALU.add)
                G("tensor_tensor", [Bsm["lnr"], Bsm["mh4"]], [Bsm["lnr"]], out=lnr, in0=lnr, in1=mh4, op=ALU.pow)
                V("scalar_tensor_tensor", [Bsm["mv"], Bsm["lnr"]], [Bsm["nmr"]], out=nmr4, in0=mv3[:, :, 0], scalar=-1.0,
                  in1=lnr, op0=ALU.mult, op1=ALU.mult)

            def B2a2(i):
                sl = i % 2
                for h in range(4):
                    A("activation", [Bhh[h], Bsm["nmr"], Bsm["lnr"]], [Btmb[sl]], out=tmb[:, sl, h * 256:(h + 1) * 256],
                      in_=hh[:, h, :], func=AF.Identity, scale=lnr[:, h:h + 1], bias=nmr4[:, h:h + 1])

            def B2b(i):
                sl = i % 2
                tsl = slice(i * 128, (i + 1) * 128)
                pt, pb_ = pnext()
                ptb = pt[:].bitcast(BF16)
                T.group("tensor", [TR(ptb[:, k * 128:(k + 1) * 128], tmb[:, sl, k * 128:(k + 1) * 128])
                                   for k in range(8)], reads=[Btmb[sl], Bidentb], writes=[pb_])
                V("tensor_tensor", [pb_, Bszg], Bhh, out=hh3, in0=k3(ptb, 8), in1=szg[:, :, tsl], op=ALU.mult)
                V("tensor_tensor", Bhh + [Bskz], [Bsz], out=sz[:, :, tsl], in0=hh3, in1=skz[:, :, tsl], op=ALU.add)

            A2(0)
            B1s(0)
            A2(1)
            B1n(0)
            B1s(1)
            B1c(0)
            B2a(0)
            for i in range(1, 4):
                if i + 1 < 4:
                    A2(i + 1)
                B1n(i)
                if i + 1 < 4:
                    B1s(i + 1)
                B1c(i)
                B2a2(i - 1)
                B2b(i - 1)
                B2a(i)
            B2a2(3)
            B2b(3)
            G("tensor_copy", [Bmx], [Bhalo], out=halo[:, :, 0:3], in_=mx[:, :, 513:516])
            if stop == "S5":
                break
            ym, Bym = sz, Bsz
            yg, Byg = gu, Bgu
            cast_some(11)
            for half in range(2):
                wgm = next_w()
                wgg = next_w()
                for (wx, dst, dbuf) in ((wgm, gmh, Bgmh), (wgg, ggh, Bggh)):
                    for m in range(4):
                        pt, pb_ = pnext()
                        mm(pt[:, :], [(wx[0][:, k, m * 128:(m + 1) * 128], hT[:, k, :]) for k in range(8)],
                           [wx[1], BhT], pb_)
                        A("activation", [pb_], [dbuf], out=dst[:, m, :], in_=pt[:, :], func=AF.Sigmoid)
                wbm = next_w()
                wbg = next_w()
                for m in range(4):
                    ch = half * 4 + m
                    t2 = (m % 2) * 2
                    pa, pab = pnext()
                    mm(pa[:, :], [(wbm[0][:, k, m * 128:(m + 1) * 128], ym[:, k, :]) for k in range(8)],
                       [wbm[1], Bym], pab)
                    pg, pgb = pnext()
                    mm(pg[:, :], [(wbg[0][:, k, m * 128:(m + 1) * 128], yg[:, k, :]) for k in range(8)],
                       [wbg[1], Byg], pgb)
                    V("tensor_tensor", [pab, Bgmh], [Bt12[t2]], out=t12[:, t2, :], in0=pa[:, :], in1=gmh[:, m, :],
                      op=ALU.mult)
                    V("tensor_tensor", [pgb, Bggh], [Bt12[t2 + 1]], out=t12[:, t2 + 1, :], in0=pg[:, :],
                      in1=ggh[:, m, :], op=ALU.mult)
                    G("tensor_tensor", [Bt12[t2], Bt12[t2 + 1]], [Bmrg], out=mrg[:, ch, :], in0=t12[:, t2, :],
                      in1=t12[:, t2 + 1, :], op=ALU.add)
            if stop == "S6m":
                break
            wo = [next_w(), next_w()]
            tm_out_stage(mrg, Bmrg, 1, lambda c, u: wo[c])
            post_norm_residual(0, True)
            if stop == "S6":
                break
            rms_to_hT(1)
            for u in range(8):
                w3, wb, _ = next_w()
                for m in range(4):
                    ch = u * 4 + m
                    sl = m % 2
                    pt, pb_ = pnext()
                    mm(pt[:, :], [(w3[:, k, m * 128:(m + 1) * 128], hT[:, k, :]) for k in range(8)], [wb, BhT], pb_)
                    A("activation", [pb_], [Bsqt[sl]], out=sqt[:, sl, :], in_=pt[:, :], func=AF.Square)
                    V("scalar_tensor_tensor", [pb_, Bsqt[sl]], [Bact[ch]], out=act[:, ch, :], in0=pt[:, :], scalar=0.0,
                      in1=sqt[:, sl, :], op0=ALU.is_gt, op1=ALU.mult)
            tm_out_stage(act, Bact, 4, lambda c, u: next_w(), per_chunk=True)
            post_norm_residual(1, True)
            if stop == "S7":
                break
            rms_to_hT(2)
            T.dma("sync", [DMA(pf32, p_d[r0:r0 + TS, :].rearrange("(i p) d -> p i d", p=128))], s_p, writes=[Bpf32])
            if sti + 1 < n_st:
                load_x(sti + 1)
            for i in range(4):
                sl = i % 2
                V("tensor_copy", [Bpf32], [Btmb[sl]], out=tmb[:, sl, 0:256], in_=pf32[:, i, :])
                pt, pb_ = pnext()
                ptb = pt[:].bitcast(BF16)
                T.group("tensor", [TR(ptb[:, k * 128:(k + 1) * 128], tmb[:, sl, k * 128:(k + 1) * 128])
                                   for k in range(2)], reads=[Btmb[sl], Bidentb], writes=[pb_])
                V("tensor_copy", [pb_], [BpT], out=pT[:, :, i * 128:(i + 1) * 128], in_=k3(ptb[:, 0:256], 2))
            wpg = [next_w(), next_w()]
            wpp = next_w()
            wpp3 = wpp[2][:, 0:2048].rearrange("p (k n) -> p k n", k=2)
            for i in range(4):
                for c in range(2):
                    pg, pgb = pnext()
                    mm(pg[:, :], [(hT[:, k, i * 128:(i + 1) * 128], wpg[c][0][:, k, :]) for k in range(8)],
                       [BhT, wpg[c][1]], pgb)
                    A("activation", [pgb], [Bsg[c]], out=sg[:, c, :], in_=pg[:, :], func=AF.Sigmoid)
                    pp, ppb = pnext()
                    mm(pp[:, :], [(pT[:, k, i * 128:(i + 1) * 128], wpp3[:, k, c * 512:(c + 1) * 512]) for k in range(2)],
                       [BpT, wpp[1]], ppb)
                    V("tensor_tensor", [ppb, Bsg[c]], [Bybuf[i]], out=ybuf[:, i, c * 512:(c + 1) * 512], in0=pp[:, :],
                      in1=sg[:, c, :], op=ALU.mult)
                A("activation", [Bybuf[i]], [Bjunk, Bsm["ss2"][i]], out=junk[:], in_=ybuf[:, i, :], func=AF.Square,
                  accum_out=ss2[:, i:i + 1])
            if sti + 1 < n_st:
                rms_to_hT(0, xnext, Bxn)
            post_norm_residual(2, False, to_ybuf=True)
            w_issue_upto(w_taken[0] + NW - 1)
            T.dma("sync", [DMA(y_d[r0 + i * 128:r0 + (i + 1) * 128, :], ybuf[:, i, :]) for i in range(4)],
                  s_st, reads=Bybuf, writes=[Bout])
        T.wait_all("sync", [Bout])
        T.replay()
    return nc


def _blockdiag(w, transpose=False):
    out = np.zeros((128, 8, 128), np.float32)
    w = np.asarray(w, np.float32).reshape(8, 32, 4, 4)
    if transpose:
        w = np.swapaxes(w, 2, 3)
    for n in range(32):
        out[4 * n:4 * n + 4, :, 4 * n:4 * n + 4] = np.transpose(w[:, n], (1, 0, 2))
    return out.reshape(128, 1024)


def _fm(v, n):
    return np.asarray(v, np.float32).reshape(n, 128).T


def host_pack(inp):
    g = lambda k: np.asarray(inp[k], np.float32)[0]
    cf = np.zeros((128, CF_N), np.float32)
    cw = g("conv_w")
    cf[:, CF_CW:CF_CW + 32] = np.transpose(cw.reshape(4, 8, 128), (2, 1, 0)).reshape(128, 32)
    cf[:, CF_CB:CF_CB + 8] = _fm(g("conv_b"), 8)
    cf[:, CF_MNG:CF_MNG + 8] = _fm(g("m_norm_g"), 8)
    cf[:, CF_SKIP:CF_SKIP + 8] = _fm(g("m_skip"), 8)
    for i, k in enumerate(("mix_pre_g", "mlp_pre_g", "ple_pre_g")):
        cf[:, CF_GPRE + 8 * i:CF_GPRE + 8 * i + 8] = _fm(g(k), 8)
    cf[:, CF_ID:CF_ID + 128] = np.eye(128, dtype=np.float32)
    cf[:, CF_TRIU:CF_TRIU + 128] = np.triu(np.ones((128, 128), np.float32))
    cf[:, CF_TRIL:CF_TRIL + 128] = np.tril(np.ones((128, 128), np.float32))
    cf[:, CF_ONES:CF_ONES + 128] = 1.0
    bd = np.stack([_blockdiag(g("wq_bd")), _blockdiag(g("wk_bd")), _blockdiag(g("wv_bd")),
                   _blockdiag(g("wv_bd"), transpose=True)], axis=1)
    wsp = np.ascontiguousarray(np.transpose(g("w_spatial"), (1, 0, 2))).reshape(128, 1024)
    wif = np.ascontiguousarray(np.transpose(g("w_if").reshape(24, 128, 8), (1, 0, 2))).reshape(128, 192)
    rows = np.zeros((7, 1024), np.float32)
    rows[0] = g("mix_post_g"); rows[1] = g("mlp_post_g"); rows[2] = g("ple_post_g")
    rows[3] = g("g_ln_w"); rows[4] = g("g_ln_b"); rows[5] = g("b_spatial").reshape(1024)
    rows[6, 0:8] = g("b_if")
    shared = {
        "w_in": np.ascontiguousarray(g("w_in")), "w_br_m": np.ascontiguousarray(g("w_br_m")),
        "w_br_g": np.ascontiguousarray(g("w_br_g")), "w_out": np.ascontiguousarray(g("w_out")),
        "w_up": np.ascontiguousarray(g("w_up")), "w_down": np.ascontiguousarray(g("w_down")),
        "w_ple_gate": np.ascontiguousarray(g("w_ple_gate")), "w_ple_proj": np.ascontiguousarray(g("w_ple_proj")),
        "cf": cf, "bd": np.ascontiguousarray(bd), "wsp": wsp, "wif": wif, "rows": rows,
    }
    return shared


def kernel(**inputs):
    x = np.asarray(inputs["x"], np.float32)
    p = np.asarray(inputs["p"], np.float32)[0]
    shared = host_pack(inputs)
    nc = build_program(8)
    in_maps = []
    for c in range(NCORE):
        m = dict(shared)
        m["x"] = np.ascontiguousarray(x[2 * c:2 * c + 2].reshape(TOK_CORE, D))
        m["p"] = np.ascontiguousarray(p[2 * c:2 * c + 2].reshape(TOK_CORE, 256))
        in_maps.append(m)
    res = run_bass_kernel_spmd(nc, in_maps, core_ids=list(range(NCORE)))
    out = np.concatenate([np.asarray(r["y"], np.float32).reshape(2, SEQ, D) for r in res.results], axis=0)
    return out
```

```python
import math
from contextlib import ExitStack
import numpy as np
import concourse.bass as bass
import concourse.mybir as mybir
from concourse.bass_utils import run_bass_kernel_spmd

F32 = mybir.dt.float32
BF16 = mybir.dt.bfloat16
AF = mybir.ActivationFunctionType
ALU = mybir.AluOpType

D = 1024
SEQ = 2048
NCORE = 8
TOK_CORE = 4096
TS = 512
EPS = 1e-6
LN16 = math.log(16.0)
NW = 4


class Buf:
    __slots__ = ("name", "lw", "rd", "al", "excl")

    def __init__(self, name):
        self.name = name
        self.lw = None
        self.rd = {}
        self.al = []
        self.excl = False


class SemC:
    def __init__(self, h, name):
        self.h = h
        self.count = 0
        self.name = name


class Eng:
    def __init__(self, name, sem):
        self.name = name
        self.sem = sem
        self.prog = []
        self.known = {}


class Tracker:
    def __init__(self, nc, stack):
        self.nc = nc
        self.stack = stack
        self.engs = {}
        for n in ("tensor", "scalar", "vector", "gpsimd", "sync"):
            self.engs[n] = Eng(n, self.new_sem("e_" + n))
        self.same_sync = {"scalar", "vector", "gpsimd"}

    def new_sem(self, name):
        h = self.stack.enter_context(self.nc.semaphore(name))
        return SemC(h, name)

    @staticmethod
    def _expand(bufs):
        flat = []

        def fl(x):
            if isinstance(x, (list, tuple)):
                for y in x:
                    fl(y)
            else:
                flat.append(x)
        fl(bufs)
        out = []
        seen = set()
        for b in flat:
            for c in [b] + b.al:
                if id(c) not in seen:
                    seen.add(id(c))
                    out.append(c)
        return out

    def _waits(self, E, reads, writes):
        deps = {}

        def add(tk):
            if tk is None:
                return
            s, v = tk
            if deps.get(s, 0) < v:
                deps[s] = v
        for b in reads:
            add(b.lw)
            if b.excl:
                for s, v in b.rd.items():
                    if s is not E.sem:
                        add((s, v))
        for b in writes:
            add(b.lw)
            for s, v in b.rd.items():
                add((s, v))
        for s, v in deps.items():
            if E.known.get(s, 0) >= v:
                continue
            if s is E.sem and E.name not in self.same_sync:
                continue
            E.prog.append(("wait", s, v))
            E.known[s] = v

    @staticmethod
    def _commit(tk, reads, writes):
        s, v = tk
        for b in writes:
            b.lw = tk
            b.rd = {}
        for b in reads:
            if b.rd.get(s, 0) < v:
                b.rd[s] = v

    def op(self, eng, emit, reads=(), writes=()):
        self.group(eng, [emit], reads, writes)

    def group(self, eng, emits, reads=(), writes=()):
        E = self.engs[eng]
        reads = self._expand(reads)
        writes = self._expand(writes)
        self._waits(E, reads, writes)
        for e in emits[:-1]:
            E.prog.append(("op", e, None, 0))
        E.sem.count += 1
        E.prog.append(("op", emits[-1], E.sem, 1))
        self._commit((E.sem, E.sem.count), reads, writes)

    def dma(self, eng, emits, sem, reads=(), writes=()):
        if not isinstance(emits, (list, tuple)):
            emits = [emits]
        E = self.engs[eng]
        reads = self._expand(reads)
        writes = self._expand(writes)
        self._waits(E, reads, writes)
        for e in emits:
            sem.count += 16
            E.prog.append(("op", e, sem, 16))
        self._commit((sem, sem.count), reads, writes)

    def wait_all(self, eng, bufs):
        E = self.engs[eng]
        bufs = self._expand(bufs)
        self._waits(E, bufs, bufs)

    def replay(self):
        nc = self.nc
        with nc.Block() as block:
            def mk(E):
                def body(e):
                    for it in E.prog:
                        if it[0] == "wait":
                            e.wait_ge(it[1].h, it[2])
                        else:
                            ins = getattr(e, it[1][0])(**it[1][1])
                            if it[2] is not None:
                                ins.then_inc(it[2].h, it[3])
                return body
            for n, E in self.engs.items():
                if E.prog:
                    getattr(block, n)(mk(E))


CF_CW = 0
CF_CB = 32
CF_MNG = 40
CF_SKIP = 48
CF_GPRE = 56
CF_ID = 80
CF_TRIU = 208
CF_TRIL = 336
CF_ONES = 464
CF_N = 592


def build_program(n_st=8, stop=None):
    nc = bass.Bass("TRN2", target_bir_lowering=False)
    ntok = n_st * TS

    def din(name, shape, dt=F32):
        return nc.dram_tensor(name, list(shape), dt, kind="ExternalInput").ap()

    x_d = din("x", [ntok, D])
    p_d = din("p", [ntok, 256])
    y_d = nc.dram_tensor("y", [ntok, D], F32, kind="ExternalOutput").ap()
    w_in_d = din("w_in", [D, 6144])
    w_brm_d = din("w_br_m", [D, D])
    w_brg_d = din("w_br_g", [D, D])
    w_out_d = din("w_out", [D, D])
    w_up_d = din("w_up", [D, 4096])
    w_down_d = din("w_down", [4096, D])
    w_pg_d = din("w_ple_gate", [D, D])
    w_pp_d = din("w_ple_proj", [256, D])
    cf_d = din("cf", [128, CF_N])
    bd_d = din("bd", [128, 4, 1024])
    wsp_d = din("wsp", [128, 1024])
    wif_d = din("wif", [128, 192])
    rows_d = din("rows", [7, 1024])

    def dscr(name, shape):
        return nc.dram_tensor(name, list(shape), BF16).ap()
    sc_in = dscr("sc_in", [12, 128, 4096])
    sc_brm = dscr("sc_brm", [2, 128, 4096])
    sc_brg = dscr("sc_brg", [2, 128, 4096])
    sc_out = dscr("sc_out", [2, 128, 4096])
    sc_up = dscr("sc_up", [8, 128, 4096])
    sc_down = dscr("sc_down", [8, 128, 4096])
    sc_pg = dscr("sc_pg", [2, 128, 4096])
    sc_pp = dscr("sc_pp", [1, 128, 2048])

    with ExitStack() as st:
        T = Tracker(nc, st)

        def sb(name, shape, dt):
            return st.enter_context(nc.sbuf_tensor("sb_" + name, list(shape), dt))

        cf = sb("cf", [128, CF_N], F32)
        bc = sb("bc", [128, 6, 1024], F32)
        bif = sb("bif", [128, 8], F32)
        wbd = sb("wbd", [128, 3, 1024], BF16)
        wifb = sb("wifb", [128, 192], BF16)
        weffv = sb("weffv", [128, 64], BF16)
        wct = sb("wct", [128, 1024], BF16)
        diagw = sb("diagw", [128, 32, 128], BF16)
        identb = sb("identb", [128, 128], BF16)
        Cf = sb("Cf", [128, 8, 257], F32)
        Cb = sb("Cb", [128, 8, 258], BF16)
        halo = sb("halo", [128, 8, 4], BF16)
        sm = sb("sm", [128, 384], F32)
        xres = sb("xres", [128, 4, 1024], F32)
        hT = sb("hT", [128, 8, 512], BF16)
        tmb = sb("tmb", [128, 2, 1024], BF16)
        junk = sb("junk", [128, 1024], BF16)
        junk2 = sb("junk2", [128, 1024], BF16)
        R1 = sb("R1", [128, 16512], BF16)
        R2 = sb("R2", [128, 16384], BF16)
        R3 = sb("R3", [128, 8192], BF16)
        hh = sb("hh", [128, 4, 256], F32)
        sts = sb("sts", [128, 8, 128], BF16)
        wsl = [sb(f"wsl{i}", [128, 4096], BF16) for i in range(NW)]
        pbank = [st.enter_context(nc.psum_tensor(f"pb{i}", [128, 512], F32)) for i in range(8)]

        _smo = [0]

        def smalloc(n):
            o = _smo[0]
            _smo[0] += n
            assert _smo[0] <= 384
            return sm[:, o:o + n]
        ss = smalloc(4); rs = smalloc(4); mh4 = smalloc(4); ss2 = smalloc(8)
        gt4 = smalloc(32).rearrange("p (i j) -> p i j", i=4)
        e14 = smalloc(16).rearrange("p (i j) -> p i j", i=4)
        lfn4 = smalloc(16).rearrange("p (i j) -> p i j", i=4)
        args4 = smalloc(80).rearrange("p (i j) -> p i j", i=4)
        EE4 = smalloc(80).rearrange("p (i j) -> p i j", i=4)
        ad = smalloc(4); mm4 = smalloc(4); r4 = smalloc(4); nmr4 = smalloc(4)
        bst = smalloc(48); mv = smalloc(8); lnr = smalloc(4); mvg = smalloc(2); lng = smalloc(2)
        bst_v = bst.rearrange("p (a b) -> p a b", b=6)

        bufs = {}
        regions = {}

        def LB(name, region=None, lo=0, hi=0):
            b = Buf(name)
            bufs[name] = b
            if region is not None:
                for (ob, olo, ohi) in regions.setdefault(region, []):
                    if lo < ohi and olo < hi:
                        b.al.append(ob)
                        ob.al.append(b)
                regions[region].append((b, lo, hi))
            return b

        mc = R1[:, 0:4096].rearrange("p (k n) -> p k n", k=8)
        sz = R1[:, 4096:8192].rearrange("p (k n) -> p k n", k=8)
        gu = R1[:, 8192:12288].rearrange("p (k n) -> p k n", k=8)
        mx = R1[:, 12288:12288 + 4128].rearrange("p (k n) -> p k n", k=8)
        act = R1[:, 0:16384].rearrange("p (k n) -> p k n", k=32)
        Bmc = [LB(f"mc{k}", "R1", k * 512, (k + 1) * 512) for k in range(8)]
        Bsz = [LB(f"sz{k}", "R1", 4096 + k * 512, 4096 + (k + 1) * 512) for k in range(8)]
        Bgu = [LB(f"gu{k}", "R1", 8192 + k * 512, 8192 + (k + 1) * 512) for k in range(8)]
        Bmx = [LB(f"mx{k}", "R1", 12288 + k * 516, 12288 + (k + 1) * 516) for k in range(8)]
        Bact = [LB(f"act{c}", "R1", c * 512, (c + 1) * 512) for c in range(32)]
        gvf = R2[:, 0:4096].bitcast(F32).rearrange("p (i d) -> p i d", i=2)
        gv = R2[:, 4096:8192].rearrange("p (i d) -> p i d", i=4)
        tmix = R2[:, 8192:9216].bitcast(F32)
        wvt_t = R2[:, 0:1024]
        wsp_t = R2[:, 1024:2048]
        qT = R2[:, 0:4096].rearrange("p (k n) -> p k n", k=8)
        kT = R2[:, 4096:8192].rearrange("p (k n) -> p k n", k=8)
        kTM = R2[:, 8192:10240].rearrange("p (i d) -> p i d", i=2)
        vt = R2[:, 10240:12320].rearrange("p (i h d) -> p i h d", i=2, h=4)
        vh = R2[:, 12320:14400].rearrange("p (i h d) -> p i h d", i=2, h=4)
        gmh = R2[:, 0:2048].rearrange("p (k n) -> p k n", k=4)
        ggh = R2[:, 2048:4096].rearrange("p (k n) -> p k n", k=4)
        mrg = R2[:, 4096:8192].rearrange("p (k n) -> p k n", k=8)
        t12 = R2[:, 8192:12288].bitcast(F32).rearrange("p (i n) -> p i n", i=4)
        sqt = R2[:, 0:2048].bitcast(F32).rearrange("p (i n) -> p i n", i=2)
        pf32 = R2[:, 0:2048].bitcast(F32).rearrange("p (i n) -> p i n", i=4)
        pT = R2[:, 2048:3072].rearrange("p (k n) -> p k n", k=2)
        sg = R2[:, 4096:6144].bitcast(F32).rearrange("p (i n) -> p i n", i=2)
        xnext = R2[:, 8192:16384].bitcast(F32).rearrange("p (i d) -> p i d", i=4)
        Bxn = [LB(f"xn{i}", "R2", 8192 + i * 2048, 8192 + (i + 1) * 2048) for i in range(4)]
        Bgvf = [LB(f"gvf{i}", "R2", i * 2048, (i + 1) * 2048) for i in range(2)]
        Bgv = [LB(f"gv{i}", "R2", 4096 + i * 1024, 4096 + (i + 1) * 1024) for i in range(4)]
        Btmix = LB("tmix", "R2", 8192, 9216)
        Bwvt = LB("wvt_t", "R2", 0, 1024); Bwsp = LB("wsp_t", "R2", 1024, 2048)
        BqT = [LB(f"qT{k}", "R2", k * 512, (k + 1) * 512) for k in range(8)]
        BkT = [LB(f"kT{k}", "R2", 4096 + k * 512, 4096 + (k + 1) * 512) for k in range(8)]
        BkTM = [LB(f"kTM{i}", "R2", 8192 + i * 1024, 8192 + (i + 1) * 1024) for i in range(2)]
        Bvt = [[LB(f"vt{i}_{h}", "R2", 10240 + i * 1040 + h * 260, 10240 + i * 1040 + (h + 1) * 260) for h in range(4)]
               for i in range(2)]
        Bvh = [[LB(f"vh{i}_{h}", "R2", 12320 + i * 1040 + h * 260, 12320 + i * 1040 + (h + 1) * 260) for h in range(4)]
               for i in range(2)]
        Bgmh = LB("gmh", "R2", 0, 2048); Bggh = LB("ggh", "R2", 2048, 4096)
        Bmrg = LB("mrg", "R2", 4096, 8192)
        Bt12 = [LB(f"t12_{i}", "R2", 8192 + i * 1024, 8192 + (i + 1) * 1024) for i in range(4)]
        Bsqt = [LB(f"sqt{i}", "R2", i * 1024, (i + 1) * 1024) for i in range(2)]
        Bpf32 = LB("pf32", "R2", 0, 2048); BpT = LB("pT", "R2", 2048, 3072)
        Bsg = [LB(f"sg{i}", "R2", 4096 + i * 1024, 4096 + (i + 1) * 1024) for i in range(2)]
        szg = R3[:, 0:4096].rearrange("p (k n) -> p k n", k=8)
        skz = R3[:, 4096:8192].rearrange("p (k n) -> p k n", k=8)
        ybuf = R3[:, 0:8192].bitcast(F32).rearrange("p (i d) -> p i d", i=4)
        Bszg = [LB(f"szg{k}", "R3", k * 512, (k + 1) * 512) for k in range(8)]
        Bskz = [LB(f"skz{k}", "R3", 4096 + k * 512, 4096 + (k + 1) * 512) for k in range(8)]
        Bybuf = [LB(f"ybuf{i}", "R3", i * 2048, (i + 1) * 2048) for i in range(4)]
        Bcf = LB("cf"); Bbc = LB("bc"); Bbif = LB("bif"); Bwbd = LB("wbd"); Bwifb = LB("wifb")
        Bweffv = LB("weffv"); Bwct = LB("wct"); Bdiagw = LB("diagw"); Bidentb = LB("identb")
        BCf = [LB(f"Cf{i}") for i in range(8)]; BCb = [LB(f"Cb{i}") for i in range(8)]
        Bhalo = LB("halo")
        Bx = [LB(f"x{i}") for i in range(4)]
        BhT = [LB(f"hT{i}") for i in range(4)]; Btmb = [LB("tmb0"), LB("tmb1")]; Bjh = [LB("junk_a"), LB("junk_b")]; Bjunk = Bjh; Bjunk2 = LB("junk2")
        Bhh = [LB(f"hh{i}") for i in range(4)]; Bsts = [LB(f"sts{i}") for i in range(8)]
        Bw = [LB(f"w{i}") for i in range(NW)]
        Bp = [LB(f"ps{i}") for i in range(8)]
        for b_ in Bp:
            b_.excl = True
        Bout = LB("out")
        Bsm = {n: LB("sm_" + n) for n in ["ss", "rs", "mh4", "ss2", "gt", "e1", "lfn", "args", "EE", "EE0", "EE1", "EE2", "EE3", "ad", "nmr",
                                            "mm4", "r4", "bst", "mv", "lnr", "mvg", "lng"]}
        NSC = {"in": 12, "brm": 2, "brg": 2, "out": 2, "up": 8, "down": 8, "pg": 2, "pp": 1}
        Bsm["ss"] = [LB(f"sm_ss{i}") for i in range(4)]
        Bsm["ss2"] = [LB(f"sm_ss2{i}") for i in range(8)]
        Bsm["ad"] = [LB(f"sm_ad{i}") for i in range(4)]
        Bsm["bst"] = [LB(f"sm_bst{i}") for i in range(8)]
        Bsc = {n: [LB(f"sc_{n}{u}") for u in range(k)] for n, k in NSC.items()}
        NCS = 6
        Bthr = [LB(f"thr{i}") for i in range(NCS)]

        s_const = T.new_sem("s_const")
        s_cast = [T.new_sem(f"s_cast{i}") for i in range(NCS)]
        s_w = [T.new_sem(f"s_w{i}") for i in range(NW)]
        s_x = T.new_sem("s_x"); s_p = T.new_sem("s_p"); s_st = T.new_sem("s_st")
        s_cc = T.new_sem("s_cc")
        s_xc = T.new_sem("s_xc")

        _pi = [0]

        pinned = set()

        def pnext():
            while True:
                i = _pi[0] % 8
                _pi[0] += 1
                if i not in pinned:
                    return pbank[i], Bp[i]

        def pin(pbuf):
            pinned.add(Bp.index(pbuf))

        def unpin(pbuf):
            pinned.discard(Bp.index(pbuf))

        def mm(out_ap, pairs, reads, pbuf, first=True, last=True):
            n = len(pairs)
            emits = []
            for i, (l, r) in enumerate(pairs):
                emits.append(("matmul", dict(out=out_ap, lhsT=l, rhs=r, start=(first and i == 0),
                                             stop=(last and i == n - 1))))
            T.group("tensor", emits, reads=reads, writes=[pbuf])

        def MM1(out, lhsT, rhs):
            return ("matmul", dict(out=out, lhsT=lhsT, rhs=rhs, start=True, stop=True))

        def TR(out, in_):
            return ("transpose", dict(out=out, in_=in_, identity=identb[:]))

        def DMA(out, in_):
            return ("dma_start", dict(out=out, in_=in_))

        def V(name, reads, writes, **kw):
            T.op("vector", (name, kw), reads, writes)

        def A(name, reads, writes, **kw):
            T.op("scalar", (name, kw), reads, writes)

        def G(name, reads, writes, **kw):
            T.op("gpsimd", (name, kw), reads, writes)

        per_st = []
        for u in (6, 7, 4, 5, 0, 1, 2, 3):
            per_st.append((sc_in[u], 4096, Bsc["in"][u]))
        for half in range(2):
            per_st.append((sc_in[8 + half], 4096, Bsc["in"][8 + half]))
            per_st.append((sc_in[10 + half], 4096, Bsc["in"][10 + half]))
            per_st.append((sc_brm[half], 4096, Bsc["brm"][half]))
            per_st.append((sc_brg[half], 4096, Bsc["brg"][half]))
        for u in range(2):
            per_st.append((sc_out[u], 4096, Bsc["out"][u]))
        for u in range(8):
            per_st.append((sc_up[u], 4096, Bsc["up"][u]))
        for u in range(8):
            per_st.append((sc_down[u], 4096, Bsc["down"][u]))
        for u in range(2):
            per_st.append((sc_pg[u], 4096, Bsc["pg"][u]))
        per_st.append((sc_pp[0], 2048, Bsc["pp"][0]))
        units = []
        for _ in range(n_st):
            units.extend(per_st)
        w_issued = [0]
        w_taken = [0]

        def w_issue_upto(n):
            n = min(n, len(units))
            while w_issued[0] < n:
                j = w_issued[0]
                ap, ncol, sbuf_ = units[j]
                slot = j % NW
                T.dma("sync", [DMA(wsl[slot][:, 0:ncol], ap)], s_w[slot], reads=[sbuf_], writes=[Bw[slot]])
                w_issued[0] += 1

        def next_w():
            j = w_taken[0]
            w_taken[0] += 1
            w_issue_upto(j + 1)
            slot = j % NW
            return wsl[slot][:, 0:4096].rearrange("p (k n) -> p k n", k=8), Bw[slot], wsl[slot]

        T.dma("sync", [DMA(cf[:], cf_d[:, :])] +
              [DMA(bc[:, r, :], rows_d[r:r + 1, :].partition_broadcast(128)) for r in range(6)] +
              [DMA(bif[:], rows_d[6:7, 0:8].partition_broadcast(128))], s_const, writes=[Bcf, Bbc, Bbif])
        T.dma("gpsimd", [DMA(wbd[:, w, :], bd_d[:, w, :]) for w in range(3)] +
              [DMA(wvt_t, bd_d[:, 3, :]), DMA(wsp_t, wsp_d[:, :]), DMA(wifb[:], wif_d[:, :])],
              s_cc, writes=[Bwbd, Bwvt, Bwsp, Bwifb])

        cast_list = []

        def cast_w(name, w_ap, scr, nunits, kind, order=None):
            for u in (order or range(nunits)):
                if kind == "cols":
                    src = w_ap.rearrange("(k p) n -> p k n", p=128)[:, :, u * 512:(u + 1) * 512]
                    dst = scr[u].rearrange("p (k n) -> p k n", k=8)
                elif kind == "down":
                    c, uu = u // 4, u % 4
                    src = w_ap.rearrange("(u k p) n -> u p k n", k=8, p=128)[uu][:, :, c * 512:(c + 1) * 512]
                    dst = scr[u].rearrange("p (k n) -> p k n", k=8)
                else:
                    src = w_ap.rearrange("(k p) n -> p k n", p=128)
                    dst = scr[0].rearrange("p (k n) -> p k n", k=2)
                cast_list.append((DMA(dst, src), Bsc[name][u]))

        cast_w("in", w_in_d, sc_in, 12, "cols", order=(6, 7, 4, 5, 0, 1, 2, 3, 8, 10, 9, 11))
        cast_w("brm", w_brm_d, sc_brm, 2, "cols")
        cast_w("brg", w_brg_d, sc_brg, 2, "cols")
        cast_w("out", w_out_d, sc_out, 2, "cols")
        cast_w("up", w_up_d, sc_up, 8, "cols")
        cast_w("down", w_down_d, sc_down, 8, "down")
        cast_w("pg", w_pg_d, sc_pg, 2, "cols")
        cast_w("pp", w_pp_d, sc_pp, 1, "pp")
        cast_done = [0]

        def cast_some(k):
            for _ in range(k):
                n = cast_done[0]
                if n >= len(cast_list):
                    return
                emit, ub = cast_list[n]
                T.dma("gpsimd", [emit], s_cast[n % NCS], writes=[ub, Bthr[n % NCS]])
                cast_done[0] += 1

        cast_some(12)

        ident_f = cf[:, CF_ID:CF_ID + 128]
        triu_f = cf[:, CF_TRIU:CF_TRIU + 128]
        tril_f = cf[:, CF_TRIL:CF_TRIL + 128]
        ones_f = cf[:, CF_ONES:CF_ONES + 128]

        def col(base, k):
            return cf[:, base + k:base + k + 1]

        def k3(ap, k):
            return ap.rearrange("p (k t) -> p k t", k=k)

        V("tensor_copy", [Bcf], [Bidentb], out=identb[:], in_=ident_f)
        G("memset", [], [Bsm["mh4"]], ap=mh4, constant=-0.5)
        for j in range(4):
            for k in range(8):
                V("tensor_scalar", [Bcf], [Bdiagw], out=diagw[:, j * 8 + k, :], in0=ident_f,
                  scalar1=col(CF_CW, k * 4 + j), scalar2=None, op0=ALU.mult)
        pt, pb_ = pnext()
        ptb = pt[:].bitcast(BF16)
        for g in range(8):
            gs = slice(g * 128, (g + 1) * 128)
            V("tensor_tensor", [Bwsp, Bcf], [Bwsp], out=wsp_t[:, gs], in0=wsp_t[:, gs], in1=tril_f, op=ALU.mult)
        T.group("tensor", [TR(ptb[:, g * 128:(g + 1) * 128], wsp_t[:, g * 128:(g + 1) * 128]) for g in range(8)],
                reads=[Bwsp, Bidentb], writes=[pb_])
        V("tensor_copy", [pb_], [Bwct], out=wct[:], in_=ptb)
        pt, pb_ = pnext()
        T.group("tensor", [MM1(pt[:, kc * 8:(kc + 1) * 8], wvt_t[:, kc * 128:(kc + 1) * 128],
                               wifb[:, (16 + kc) * 8:(17 + kc) * 8]) for kc in range(8)],
                reads=[Bwvt, Bwifb], writes=[pb_])
        V("tensor_copy", [pb_], [Bweffv], out=weffv[:], in_=pt[:, 0:64])

        Wq = k3(wbd[:, 0, :], 8)
        Wk = k3(wbd[:, 1, :], 8)
        Wv = k3(wbd[:, 2, :], 8)
        wif3 = wifb[:].rearrange("p (c j) -> p c j", j=8)
        weff3 = weffv[:].rearrange("p (c j) -> p c j", j=8)
        wct3 = k3(wct[:], 8)
        bsp3 = k3(bc[:, 5, :], 8)
        s3 = ss2.rearrange("p (i c) -> p i c", c=2)
        mv3 = mv.rearrange("p (h c) -> p h c", c=2)
        hh3 = k3(hh[:].rearrange("p h d -> p (h d)"), 8)
        tmix3 = k3(tmix, 4)

        def rms_to_hT(gidx, xsrc=None, Bsrc=None):
            if xsrc is None:
                xsrc, Bsrc = xres, Bx
            for i in (0, 2, 1, 3):
                if i < 2:
                    A("activation", [Bsrc[i]], [Bjunk, Bsm["ss"][i]], out=junk[:], in_=xsrc[:, i, :], func=AF.Square,
                      accum_out=ss[:, i:i + 1])
                else:
                    V("scalar_tensor_tensor", [Bsrc[i]], [Bjunk2, Bsm["ss"][i]], out=junk2[:], in0=xsrc[:, i, :], scalar=1.0,
                      in1=xsrc[:, i, :], op0=ALU.mult, op1=ALU.mult, accum_out=ss[:, i:i + 1])
            V("tensor_scalar", [Bsm["ss"]], [Bsm["rs"]], out=rs, in0=ss, scalar1=1.0 / D, scalar2=EPS,
              op0=ALU.mult, op1=ALU.add)
            G("tensor_tensor", [Bsm["rs"], Bsm["mh4"]], [Bsm["rs"]], out=rs, in0=rs, in1=mh4, op=ALU.pow)
            gcol = CF_GPRE + gidx * 8
            gb = cf[:, gcol:gcol + 8].unsqueeze(2).to_broadcast([128, 8, 128])
            for i in range(4):
                sl = i % 2
                A("activation", [Bsrc[i], Bsm["rs"]], [Btmb[sl]], out=tmb[:, sl, :], in_=xsrc[:, i, :], func=AF.Copy,
                  scale=rs[:, i:i + 1])
                pt, pb_ = pnext()
                ptb = pt[:].bitcast(BF16)
                T.group("tensor", [TR(ptb[:, k * 128:(k + 1) * 128], tmb[:, sl, k * 128:(k + 1) * 128])
                                   for k in range(8)], reads=[Btmb[sl], Bidentb], writes=[pb_])
                V("tensor_tensor", [pb_, Bcf], [BhT[i]], out=hT[:, :, i * 128:(i + 1) * 128], in0=k3(ptb, 8), in1=gb,
                  op=ALU.mult)

        def post_norm_residual(pidx, halves, to_ybuf=False):
            if halves:
                V("tensor_tensor", [Bsm["ss2"]], [Bsm["ss"]], out=ss, in0=s3[:, :, 0], in1=s3[:, :, 1], op=ALU.add)
                src, bsrc = ss, Bsm["ss"]
            else:
                src, bsrc = ss2[:, 0:4], Bsm["ss2"]
            V("tensor_scalar", [bsrc], [Bsm["rs"]], out=rs, in0=src, scalar1=1.0 / D, scalar2=EPS,
              op0=ALU.mult, op1=ALU.add)
            G("tensor_tensor", [Bsm["rs"], Bsm["mh4"]], [Bsm["rs"]], out=rs, in0=rs, in1=mh4, op=ALU.pow)
            for i in range(4):
                V("scalar_tensor_tensor", [Bybuf[i], Bsm["rs"], Bbc], [Bybuf[i]], out=ybuf[:, i, :],
                  in0=ybuf[:, i, :], scalar=rs[:, i:i + 1], in1=bc[:, pidx, :], op0=ALU.mult, op1=ALU.mult)
                dst, dbuf = (ybuf[:, i, :], Bybuf[i]) if to_ybuf else (xres[:, i, :], Bx[i])
                V("tensor_tensor", [Bx[i], Bybuf[i]], [dbuf], out=dst, in0=xres[:, i, :], in1=ybuf[:, i, :], op=ALU.add)

        def tm_out_stage(lhs_tile, lhs_buf, nk_units, get_rhs, per_chunk=False):
            for c in range(2):
                banks = [pnext() for _ in range(4)]
                for u in range(nk_units):
                    w3, wb, _ = get_rhs(c, u)
                    for i in range(4):
                        pt, pb_ = banks[i]
                        mm(pt[:, :], [(lhs_tile[:, u * 8 + k, i * 128:(i + 1) * 128], w3[:, k, :]) for k in range(8)],
                           ([lhs_buf[u * 8 + k] for k in range(8)] if per_chunk else [lhs_buf]) + [wb], pb_,
                           first=(u == 0), last=(u == nk_units - 1))
                for i in range(4):
                    pt, pb_ = banks[i]
                    V("tensor_copy", [pb_], [Bybuf[i]], out=ybuf[:, i, c * 512:(c + 1) * 512], in_=pt[:, :])
                    A("activation", [Bybuf[i]], [Bjh[i % 2], Bsm["ss2"][i * 2 + c]], out=junk[:, (i % 2) * 512:(i % 2 + 1) * 512],
                      in_=ybuf[:, i, c * 512:(c + 1) * 512], func=AF.Square,
                      accum_out=ss2[:, i * 2 + c:i * 2 + c + 1])

        def load_x(sti_):
            rr = sti_ * TS
            T.dma("sync", [DMA(xnext[:, i, :], x_d[rr + i * 128:rr + (i + 1) * 128, :]) for i in range(4)],
                  s_x, writes=Bxn)

        if stop != "P":
            w_issue_upto(NW - 1)
            load_x(0)
            rms_to_hT(0, xnext, Bxn)
        for sti in range(0 if stop == "P" else n_st):
            first_of_seq = (sti % 4 == 0)
            r0 = sti * TS
            T.dma("gpsimd", [DMA(xres[:, i, :], xnext[:, i, :]) for i in range(4)], s_xc, reads=Bxn, writes=Bx)
            if first_of_seq:
                G("memset", [], BCf, ap=Cf[:], constant=0.0)
                G("memset", [], BCb, ap=Cb[:], constant=0.0)
                G("memset", [], [Bhalo], ap=halo[:], constant=0.0)
            cast_some(6)
            wvs = [next_w(), next_w()]

            def gv_mm(i):
                sl = i % 2
                for c in range(2):
                    pt, pb_ = pnext()
                    mm(pt[:, :], [(hT[:, k, i * 128:(i + 1) * 128], wvs[c][0][:, k, :]) for k in range(8)],
                       [BhT, wvs[c][1]], pb_)
                    A("activation", [pb_], [Bgvf[sl]], out=gvf[:, sl, c * 512:(c + 1) * 512], in_=pt[:, :],
                      func=AF.Gelu_apprx_tanh)

            def gv_ln(i):
                sl = i % 2
                for c in range(2):
                    V("bn_stats", [Bgvf[sl]], [Bsm["bst"][c]], out=bst_v[:, c, :], in_=gvf[:, sl, c * 512:(c + 1) * 512])
                V("bn_aggr", Bsm["bst"][0:2], [Bsm["mvg"]], out=mvg, in_=bst[:, 0:12])
                V("tensor_scalar", [Bsm["mvg"]], [Bsm["lng"]], out=lng[:, 0:1], in0=mvg[:, 1:2], scalar1=EPS,
                  scalar2=None, op0=ALU.add)
                G("tensor_tensor", [Bsm["lng"], Bsm["mh4"]], [Bsm["lng"]], out=lng[:, 0:1], in0=lng[:, 0:1],
                  in1=mh4[:, 0:1], op=ALU.pow)
                V("tensor_scalar", [Bgvf[sl], Bsm["mvg"], Bsm["lng"]], [Bgvf[sl]], out=gvf[:, sl, :], in0=gvf[:, sl, :],
                  scalar1=mvg[:, 0:1], scalar2=lng[:, 0:1], op0=ALU.subtract, op1=ALU.mult)
                V("tensor_tensor", [Bgvf[sl], Bbc], [Bgvf[sl]], out=gvf[:, sl, :], in0=gvf[:, sl, :], in1=bc[:, 3, :],
                  op=ALU.mult)
                V("tensor_tensor", [Bgvf[sl], Bbc], [Bgv[i]], out=gv[:, i, :], in0=gvf[:, sl, :], in1=bc[:, 4, :],
                  op=ALU.add)

            def gv_mix(i):
                for hf in range(2):
                    pt, pb_ = pnext()
                    T.group("tensor", [MM1(pt[:, j * 128:(j + 1) * 128],
                                           gv[:, i, (hf * 4 + j) * 128:(hf * 4 + j + 1) * 128],
                                           wct3[:, hf * 4 + j, :]) for j in range(4)],
                            reads=[Bgv[i], Bwct], writes=[pb_])
                    V("tensor_tensor", [pb_, Bbc], [Btmix], out=tmix3, in0=k3(pt[:, :], 4),
                      in1=bsp3[:, hf * 4:(hf + 1) * 4, :], op=ALU.add)
                    gus = gu[:, hf * 4:(hf + 1) * 4, i * 128:(i + 1) * 128]
                    V("tensor_tensor", [Btmix, Bgu[hf * 4:(hf + 1) * 4]], [Bgu[hf * 4:(hf + 1) * 4]], out=gus, in0=tmix3, in1=gus, op=ALU.mult)

            def fm_unit(u):
                w3, wb, _ = next_w()
                grp = u // 2
                for m in range(4):
                    ch = (u % 2) * 4 + m
                    pt, pb_ = pnext()
                    mm(pt[:, :], [(w3[:, k, m * 128:(m + 1) * 128], hT[:, k, :]) for k in range(8)], [wb, BhT], pb_)
                    if grp == 0:
                        A("activation", [pb_], [Bmx[ch]], out=mx[:, ch, 4:516], in_=pt[:, :], func=AF.Identity)
                    elif grp == 1:
                        A("activation", [pb_], [Bsz[ch]], out=sz[:, ch, :], in_=pt[:, :], func=AF.Silu)
                    else:
                        A("activation", [pb_], [Bgu[ch]], out=gu[:, ch, :], in_=pt[:, :], func=AF.Gelu_apprx_tanh)

            gv_mm(0); gv_mm(1)
            fm_unit(4)
            gv_ln(0); gv_ln(1)
            gv_mm(2); gv_mm(3)
            fm_unit(5)
            gv_ln(2); gv_ln(3)
            fm_unit(0)
            gv_mix(0); gv_mix(1)
            fm_unit(1)
            gv_mix(2); gv_mix(3)
            fm_unit(2)
            fm_unit(3)
            G("tensor_copy", [Bhalo], [Bmx], out=mx[:, :, 1:4], in_=halo[:, :, 0:3])
            for k in range(8):
                pt, pb_ = pnext()
                mm(pt[:, :], [(diagw[:, j * 8 + k, :], mx[:, k, 1 + j:513 + j]) for j in range(4)], [Bdiagw, Bmx[k]], pb_)
                A("activation", [pb_, Bcf], [Bmc[k]], out=mc[:, k, :], in_=pt[:, :], func=AF.Silu, bias=col(CF_CB, k))
            if stop == "S4":
                break
            cast_some(8)
            for k in range(8):
                pt, pb_ = pnext()
                mm(pt[:, :], [(Wq[:, k, :], mc[:, k, :])], [Bwbd, Bmc[k]], pb_)
                V("tensor_copy", [pb_], [BqT[k]], out=qT[:, k, :], in_=pt[:, :])
                pt, pb_ = pnext()
                mm(pt[:, :], [(Wk[:, k, :], mc[:, k, :])], [Bwbd, Bmc[k]], pb_)
                A("activation", [pb_], [BkT[k]], out=kT[:, k, :], in_=pt[:, :], func=AF.Identity)
            for k in range(8):
                V("tensor_scalar", [Bsz[k], Bcf], [Bszg[k]], out=szg[:, k, :], in0=sz[:, k, :], scalar1=col(CF_MNG, k),
                  scalar2=None, op0=ALU.mult)
                V("scalar_tensor_tensor", [Bmc[k], Bsz[k], Bcf], [Bskz[k]], out=skz[:, k, :], in0=mc[:, k, :],
                  scalar=col(CF_SKIP, k), in1=sz[:, k, :], op0=ALU.mult, op1=ALU.mult)
            pt, pb_ = pnext()
            for i in range(4):
                tsl = slice(i * 128, (i + 1) * 128)
                xsl = slice(4 + i * 128, 4 + (i + 1) * 128)
                pairs = [(qT[:, kc, tsl], wif3[:, kc, :]) for kc in range(8)]
                pairs += [(kT[:, kc, tsl], wif3[:, 8 + kc, :]) for kc in range(8)]
                pairs += [(mx[:, kc, xsl], weff3[:, kc, :]) for kc in range(8)]
                mm(pt[:, i * 8:(i + 1) * 8], pairs, [BqT, BkT, Bmx, Bwifb, Bweffv], pb_)
            V("tensor_tensor", [pb_, Bbif], [Bsm["gt"]], out=gt4, in0=pt[:, 0:32].rearrange("p (i j) -> p i j", i=4),
              in1=bif[:].unsqueeze(1).to_broadcast([128, 4, 8]), op=ALU.add)
            A("activation", [Bsm["gt"]], [Bsm["e1"]], out=e14, in_=gt4[:, :, 4:8], func=AF.Exp, scale=-1.0)
            A("activation", [Bsm["e1"]], [Bsm["lfn"]], out=lfn4, in_=e14, func=AF.Ln, bias=1.0)
            pt2, pb2 = pnext()
            T.group("tensor", [MM1(pt2[:, i * 8:i * 8 + 4], triu_f, lfn4[:, i, :]) for i in range(4)] +
                    [MM1(pt2[:, i * 8 + 4:i * 8 + 8], ones_f, lfn4[:, i, :]) for i in range(4)],
                    reads=[Bcf, Bsm["lfn"]], writes=[pb2])
            p2v = pt2[:, 0:32].rearrange("p (i j) -> p i j", i=4)
            Ba = Bsm["args"]
            V("tensor_scalar", [pb2], [Ba], out=args4[:, :, 0:4], in0=p2v[:, :, 0:4], scalar1=-1.0, scalar2=None,
              op0=ALU.mult)
            V("scalar_tensor_tensor", [pb2, Bsm["gt"]], [Ba], out=args4[:, :, 4:8], in0=gt4[:, :, 0:4], scalar=-LN16,
              in1=p2v[:, :, 0:4], op0=ALU.add, op1=ALU.add)
            V("tensor_tensor", [pb2, Ba], [Ba], out=args4[:, :, 8:12], in0=args4[:, :, 4:8], in1=p2v[:, :, 4:8],
              op=ALU.subtract)
            V("tensor_scalar", [pb2], [Ba], out=args4[:, :, 12:16], in0=p2v[:, :, 4:8], scalar1=-1.0, scalar2=None,
              op0=ALU.mult)
            V("tensor_copy", [pb2], [Ba], out=args4[:, :, 16:20], in_=p2v[:, :, 0:4])
            A("activation", [Ba], [Bsm["EE"]], out=EE4, in_=args4, func=AF.Exp)
            BE = Bsm["EE"]
            if stop == "S5a":
                break

            def A2(i):
                sl = i % 2
                EE = EE4[:, i, :]
                tsl = slice(i * 128, (i + 1) * 128)
                xsl = slice(4 + i * 128, 4 + (i + 1) * 128)
                for hf in range(2):
                    pt, pb_ = pnext()
                    T.group("tensor", [MM1(pt[:, j * 128:(j + 1) * 128], mc[:, hf * 4 + j, tsl], Wk[:, hf * 4 + j, :])
                                       for j in range(4)], reads=[Bmc, Bwbd], writes=[pb_])
                    A("activation", [pb_], [BkTM[sl]], out=kTM[:, sl, hf * 512:(hf + 1) * 512], in_=pt[:, :],
                      func=AF.Identity)
                    pt, pb_ = pnext()
                    T.group("tensor", [MM1(pt[:, j * 128:(j + 1) * 128], mx[:, hf * 4 + j, xsl], Wv[:, hf * 4 + j, :])
                                       for j in range(4)], reads=[Bmx, Bwbd], writes=[pb_])
                    for hl in range(2):
                        h = hf * 2 + hl
                        A("activation", [pb_, BE], [Bvt[sl][h]], out=vt[:, sl, h, 0:256],
                          in_=pt[:, hl * 256:(hl + 1) * 256], func=AF.Copy, scale=EE[:, 4 + h:5 + h])
                    for hl in range(2):
                        h = hf * 2 + hl
                        A("activation", [pb_, BE], [Bvh[sl][h]], out=vh[:, sl, h, 0:256],
                          in_=pt[:, hl * 256:(hl + 1) * 256], func=AF.Copy, scale=EE[:, 8 + h:9 + h])
                V("tensor_copy", [BE], Bvt[sl], out=vt[:, sl, :, 256:257], in_=EE[:, 4:8].unsqueeze(2))
                V("tensor_copy", [BE], Bvh[sl], out=vh[:, sl, :, 256:257], in_=EE[:, 8:12].unsqueeze(2))

            pN_of = {}

            def B1s(i):
                tsl = slice(i * 128, (i + 1) * 128)
                pS = []
                for h in range(4):
                    p_, pb_ = pnext()
                    mm(p_[:, 0:128], [(kT[:, 2 * h + c, tsl], qT[:, 2 * h + c, tsl]) for c in range(2)],
                       [BkT[2 * h:2 * h + 2], BqT[2 * h:2 * h + 2]], pb_)
                    pS.append((p_, pb_))
                for h in range(4):
                    sh = (i % 2) * 4 + h
                    V("tensor_tensor", [pS[h][1], Bcf], [Bsts[sh]], out=sts[:, sh, :], in0=pS[h][0][:, 0:128], in1=triu_f,
                      op=ALU.mult)

            def B1n(i):
                sl = i % 2
                tsl = slice(i * 128, (i + 1) * 128)
                pN = []
                for h in range(4):
                    sh = (i % 2) * 4 + h
                    pn, pnb = pnext()
                    mm(pn[:, 0:257], [(qT[:, 2 * h, tsl], Cb[:, 2 * h, 0:257]),
                                      (qT[:, 2 * h + 1, tsl], Cb[:, 2 * h + 1, 0:257]),
                                      (sts[:, sh, :], vt[:, sl, h, 0:257])],
                       [BqT[2 * h:2 * h + 2], BCb[2 * h], BCb[2 * h + 1], Bsts[sh], Bvt[sl][h]], pnb)
                    pN.append((pn, pnb))
                    pin(pnb)
                pN_of[i] = pN
                for h in range(4):
                    A("activation", [pN[h][1]], [Bsm["ad"][h]], out=ad[:, h:h + 1], in_=pN[h][0][:, 256:257], func=AF.Abs)

            def B1c(i):
                sl = i % 2
                EE = EE4[:, i, :]
                for h in range(4):
                    for c in range(2):
                        ci = 2 * h + c
                        pC, pCb = pnext()
                        mm(pC[:, 0:257], [(kTM[:, sl, ci * 128:(ci + 1) * 128], vh[:, sl, h, 0:257])],
                           [BkTM[sl], Bvh[sl][h]], pCb)
                        V("scalar_tensor_tensor", [pCb, BCf[ci], BE], [BCf[ci]], out=Cf[:, ci, :], in0=Cf[:, ci, :],
                          scalar=EE[:, 12 + h:13 + h], in1=pC[:, 0:257], op0=ALU.mult, op1=ALU.add)
                        A("activation", [BCf[ci]], [BCb[ci]], out=Cb[:, ci, 0:257], in_=Cf[:, ci, :], func=AF.Copy)

            def B2a(i):
                sl = i % 2
                EE = EE4[:, i, :]
                pN = pN_of[i]
                V("tensor_tensor", [Bsm["ad"], BE], [Bsm["mm4"]], out=mm4, in0=ad, in1=EE[:, 16:20], op=ALU.max)
                V("reciprocal", [Bsm["mm4"]], [Bsm["r4"]], out=r4, in_=mm4)
                for h in range(4):
                    pn, pnb = pN[h]
                    A("activation", [pnb, Bsm["r4"]], [Bhh[h]], out=hh[:, h, :], in_=pn[:, 0:256], func=AF.Copy,
                      scale=r4[:, h:h + 1])
                    unpin(pnb)
                for h in range(4):
                    V("bn_stats", [Bhh[h]], [Bsm["bst"][h]], out=bst_v[:, h, :], in_=hh[:, h, :])
                for h in range(4):
                    V("bn_aggr", [Bsm["bst"][h]], [Bsm["mv"]], out=mv[:, 2 * h:2 * h + 2], in_=bst[:, 6 * h:6 * h + 6])
                V("tensor_scalar", [Bsm["mv"]], [Bsm["lnr"]], out=lnr, in0=mv3[:, :, 1], scalar1=EPS, scalar2=None,
                  op0=ALU.add)
                G("tensor_tensor", [Bsm["lnr"], Bsm["mh4"]], [Bsm["lnr"]], out=lnr, in0=lnr, in1=mh4, op=ALU.pow)
                V("scalar_tensor_tensor", [Bsm["mv"], Bsm["lnr"]], [Bsm["nmr"]], out=nmr4, in0=mv3[:, :, 0], scalar=-1.0,
                  in1=lnr, op0=ALU.mult, op1=ALU.mult)

            def B2a2(i):
                sl = i % 2
                for h in range(4):
                    A("activation", [Bhh[h], Bsm["nmr"], Bsm["lnr"]], [Btmb[sl]], out=tmb[:, sl, h * 256:(h + 1) * 256],
                      in_=hh[:, h, :], func=AF.Identity, scale=lnr[:, h:h + 1], bias=nmr4[:, h:h + 1])

            def B2b(i):
                sl = i % 2
                tsl = slice(i * 128, (i + 1) * 128)
                pt, pb_ = pnext()
                ptb = pt[:].bitcast(BF16)
                T.group("tensor", [TR(ptb[:, k * 128:(k + 1) * 128], tmb[:, sl, k * 128:(k + 1) * 128])
                                   for k in range(8)], reads=[Btmb[sl], Bidentb], writes=[pb_])
                V("tensor_tensor", [pb_, Bszg], Bhh, out=hh3, in0=k3(ptb, 8), in1=szg[:, :, tsl], op=ALU.mult)
                V("tensor_tensor", Bhh + [Bskz], [Bsz], out=sz[:, :, tsl], in0=hh3, in1=skz[:, :, tsl], op=ALU.add)

            A2(0)
            B1s(0)
            A2(1)
            B1n(0)
            B1s(1)
            B1c(0)
            B2a(0)
            for i in range(1, 4):
                if i + 1 < 4:
                    A2(i + 1)
                B1n(i)
                if i + 1 < 4:
                    B1s(i + 1)
                B1c(i)
                B2a2(i - 1)
                B2b(i - 1)
                B2a(i)
            B2a2(3)
            B2b(3)
            G("tensor_copy", [Bmx], [Bhalo], out=halo[:, :, 0:3], in_=mx[:, :, 513:516])
            if stop == "S5":
                break
            ym, Bym = sz, Bsz
            yg, Byg = gu, Bgu
            cast_some(11)
            for half in range(2):
                wgm = next_w()
                wgg = next_w()
                for (wx, dst, dbuf) in ((wgm, gmh, Bgmh), (wgg, ggh, Bggh)):
                    for m in range(4):
                        pt, pb_ = pnext()
                        mm(pt[:, :], [(wx[0][:, k, m * 128:(m + 1) * 128], hT[:, k, :]) for k in range(8)],
                           [wx[1], BhT], pb_)
                        A("activation", [pb_], [dbuf], out=dst[:, m, :], in_=pt[:, :], func=AF.Sigmoid)
                wbm = next_w()
                wbg = next_w()
                for m in range(4):
                    ch = half * 4 + m
                    t2 = (m % 2) * 2
                    pa, pab = pnext()
                    mm(pa[:, :], [(wbm[0][:, k, m * 128:(m + 1) * 128], ym[:, k, :]) for k in range(8)],
                       [wbm[1], Bym], pab)
                    pg, pgb = pnext()
                    mm(pg[:, :], [(wbg[0][:, k, m * 128:(m + 1) * 128], yg[:, k, :]) for k in range(8)],
                       [wbg[1], Byg], pgb)
                    V("tensor_tensor", [pab, Bgmh], [Bt12[t2]], out=t12[:, t2, :], in0=pa[:, :], in1=gmh[:, m, :],
                      op=ALU.mult)
                    V("tensor_tensor", [pgb, Bggh], [Bt12[t2 + 1]], out=t12[:, t2 + 1, :], in0=pg[:, :],
                      in1=ggh[:, m, :], op=ALU.mult)
                    V("tensor_tensor", [Bt12[t2], Bt12[t2 + 1]], [Bmrg], out=mrg[:, ch, :], in0=t12[:, t2, :],
                      in1=t12[:, t2 + 1, :], op=ALU.add)
            if stop == "S6m":
                break
            wo = [next_w(), next_w()]
            tm_out_stage(mrg, Bmrg, 1, lambda c, u: wo[c])
            post_norm_residual(0, True)
            if stop == "S6":
                break
            rms_to_hT(1)
            for u in range(8):
                w3, wb, _ = next_w()
                for m in range(4):
                    ch = u * 4 + m
                    sl = m % 2
                    pt, pb_ = pnext()
                    mm(pt[:, :], [(w3[:, k, m * 128:(m + 1) * 128], hT[:, k, :]) for k in range(8)], [wb, BhT], pb_)
                    A("activation", [pb_], [Bsqt[sl]], out=sqt[:, sl, :], in_=pt[:, :], func=AF.Square)
                    V("scalar_tensor_tensor", [pb_, Bsqt[sl]], [Bact[ch]], out=act[:, ch, :], in0=pt[:, :], scalar=0.0,
                      in1=sqt[:, sl, :], op0=ALU.is_gt, op1=ALU.mult)
            tm_out_stage(act, Bact, 4, lambda c, u: next_w(), per_chunk=True)
            post_norm_residual(1, True)
            if stop == "S7":
                break
            rms_to_hT(2)
            T.dma("sync", [DMA(pf32, p_d[r0:r0 + TS, :].rearrange("(i p) d -> p i d", p=128))], s_p, writes=[Bpf32])
            if sti + 1 < n_st:
                load_x(sti + 1)
            for i in range(4):
                sl = i % 2
                V("tensor_copy", [Bpf32], [Btmb[sl]], out=tmb[:, sl, 0:256], in_=pf32[:, i, :])
                pt, pb_ = pnext()
                ptb = pt[:].bitcast(BF16)
                T.group("tensor", [TR(ptb[:, k * 128:(k + 1) * 128], tmb[:, sl, k * 128:(k + 1) * 128])
                                   for k in range(2)], reads=[Btmb[sl], Bidentb], writes=[pb_])
                V("tensor_copy", [pb_], [BpT], out=pT[:, :, i * 128:(i + 1) * 128], in_=k3(ptb[:, 0:256], 2))
            wpg = [next_w(), next_w()]
            wpp = next_w()
            wpp3 = wpp[2][:, 0:2048].rearrange("p (k n) -> p k n", k=2)
            for i in range(4):
                for c in range(2):
                    pg, pgb = pnext()
                    mm(pg[:, :], [(hT[:, k, i * 128:(i + 1) * 128], wpg[c][0][:, k, :]) for k in range(8)],
                       [BhT, wpg[c][1]], pgb)
                    A("activation", [pgb], [Bsg[c]], out=sg[:, c, :], in_=pg[:, :], func=AF.Sigmoid)
                    pp, ppb = pnext()
                    mm(pp[:, :], [(pT[:, k, i * 128:(i + 1) * 128], wpp3[:, k, c * 512:(c + 1) * 512]) for k in range(2)],
                       [BpT, wpp[1]], ppb)
                    V("tensor_tensor", [ppb, Bsg[c]], [Bybuf[i]], out=ybuf[:, i, c * 512:(c + 1) * 512], in0=pp[:, :],
                      in1=sg[:, c, :], op=ALU.mult)
                A("activation", [Bybuf[i]], [Bjunk, Bsm["ss2"][i]], out=junk[:], in_=ybuf[:, i, :], func=AF.Square,
                  accum_out=ss2[:, i:i + 1])
            if sti + 1 < n_st:
                rms_to_hT(0, xnext, Bxn)
            post_norm_residual(2, False, to_ybuf=True)
            w_issue_upto(w_taken[0] + NW - 1)
            T.dma("sync", [DMA(y_d[r0 + i * 128:r0 + (i + 1) * 128, :], ybuf[:, i, :]) for i in range(4)],
                  s_st, reads=Bybuf, writes=[Bout])
        T.wait_all("sync", [Bout])
        T.replay()
    return nc


def _blockdiag(w, transpose=False):
    out = np.zeros((128, 8, 128), np.float32)
    w = np.asarray(w, np.float32).reshape(8, 32, 4, 4)
    if transpose:
        w = np.swapaxes(w, 2, 3)
    for n in range(32):
        out[4 * n:4 * n + 4, :, 4 * n:4 * n + 4] = np.transpose(w[:, n], (1, 0, 2))
    return out.reshape(128, 1024)


def _fm(v, n):
    return np.asarray(v, np.float32).reshape(n, 128).T


def host_pack(inp):
    g = lambda k: np.asarray(inp[k], np.float32)[0]
    cf = np.zeros((128, CF_N), np.float32)
    cw = g("conv_w")
    cf[:, CF_CW:CF_CW + 32] = np.transpose(cw.reshape(4, 8, 128), (2, 1, 0)).reshape(128, 32)
    cf[:, CF_CB:CF_CB + 8] = _fm(g("conv_b"), 8)
    cf[:, CF_MNG:CF_MNG + 8] = _fm(g("m_norm_g"), 8)
    cf[:, CF_SKIP:CF_SKIP + 8] = _fm(g("m_skip"), 8)
    for i, k in enumerate(("mix_pre_g", "mlp_pre_g", "ple_pre_g")):
        cf[:, CF_GPRE + 8 * i:CF_GPRE + 8 * i + 8] = _fm(g(k), 8)
    cf[:, CF_ID:CF_ID + 128] = np.eye(128, dtype=np.float32)
    cf[:, CF_TRIU:CF_TRIU + 128] = np.triu(np.ones((128, 128), np.float32))
    cf[:, CF_TRIL:CF_TRIL + 128] = np.tril(np.ones((128, 128), np.float32))
    cf[:, CF_ONES:CF_ONES + 128] = 1.0
    bd = np.stack([_blockdiag(g("wq_bd")), _blockdiag(g("wk_bd")), _blockdiag(g("wv_bd")),
                   _blockdiag(g("wv_bd"), transpose=True)], axis=1)
    wsp = np.ascontiguousarray(np.transpose(g("w_spatial"), (1, 0, 2))).reshape(128, 1024)
    wif = np.ascontiguousarray(np.transpose(g("w_if").reshape(24, 128, 8), (1, 0, 2))).reshape(128, 192)
    rows = np.zeros((7, 1024), np.float32)
    rows[0] = g("mix_post_g"); rows[1] = g("mlp_post_g"); rows[2] = g("ple_post_g")
    rows[3] = g("g_ln_w"); rows[4] = g("g_ln_b"); rows[5] = g("b_spatial").reshape(1024)
    rows[6, 0:8] = g("b_if")
    shared = {
        "w_in": np.ascontiguousarray(g("w_in")), "w_br_m": np.ascontiguousarray(g("w_br_m")),
        "w_br_g": np.ascontiguousarray(g("w_br_g")), "w_out": np.ascontiguousarray(g("w_out")),
        "w_up": np.ascontiguousarray(g("w_up")), "w_down": np.ascontiguousarray(g("w_down")),
        "w_ple_gate": np.ascontiguousarray(g("w_ple_gate")), "w_ple_proj": np.ascontiguousarray(g("w_ple_proj")),
        "cf": cf, "bd": np.ascontiguousarray(bd), "wsp": wsp, "wif": wif, "rows": rows,
    }
    return shared


def kernel(**inputs):
    x = np.asarray(inputs["x"], np.float32)
    p = np.asarray(inputs["p"], np.float32)[0]
    shared = host_pack(inputs)
    nc = build_program(8)
    in_maps = []
    for c in range(NCORE):
        m = dict(shared)
        m["x"] = np.ascontiguousarray(x[2 * c:2 * c + 2].reshape(TOK_CORE, D))
        m["p"] = np.ascontiguousarray(p[2 * c:2 * c + 2].reshape(TOK_CORE, 256))
        in_maps.append(m)
    res = run_bass_kernel_spmd(nc, in_maps, core_ids=list(range(NCORE)))
    out = np.concatenate([np.asarray(r["y"], np.float32).reshape(2, SEQ, D) for r in res.results], axis=0)
    return out
```
